# Optimizing a Trainium2 kernel written in Bass

```python
import jax, jax.numpy as jnp
from jax import lax
import numpy as np

D_MODEL = 2048
BATCH = 4
SEQ = 8192
DEPTH = 1

POOL_WIDTH = D_MODEL // 2
POOL_WINDOWS = (2, 4, 8, 16)
POOL_GROUP = POOL_WIDTH // len(POOL_WINDOWS)
LRU_WIDTH = D_MODEL // 2
LRU_HEADS = 8
LRU_HEAD_DIM = LRU_WIDTH // LRU_HEADS
CONV_WIDTH = 4
LRU_C = 8.0
MIX_WIDTH = POOL_WIDTH + LRU_WIDTH
IN_PROJ_WIDTH = POOL_WIDTH + 2 * LRU_WIDTH
PEER_HEADS = 8
PEER_N_KEYS = 128
PEER_N_EXPERTS = PEER_N_KEYS * PEER_N_KEYS
PEER_D_KEY = 256
PEER_HALF = PEER_D_KEY // 2
PEER_TOPK = 16
PEER_CHUNK = 128
EPS = 1e-6

kernel_name = "hybrid_pool_rglru_peer_block"


def rmsnorm(x, g):
    xf = x.astype(jnp.float32)
    y = xf * lax.rsqrt(jnp.mean(xf * xf, axis=-1, keepdims=True) + EPS)
    return (y * g.astype(jnp.float32)).astype(x.dtype)


def pool_mixer(u, pool_w, pool_b, pool_scale):
    B, S, _ = u.shape
    uf = u.astype(jnp.float32).reshape(B, S, len(POOL_WINDOWS), POOL_GROUP)
    csum = jnp.cumsum(uf, axis=1)
    pos = jnp.arange(1, S + 1, dtype=jnp.float32)[None, :, None]
    outs = []
    for g, w in enumerate(POOL_WINDOWS):
        c = csum[:, :, g]
        c_prev = jnp.pad(c, ((0, 0), (w, 0), (0, 0)))[:, :S]
        mean = (c - c_prev) / jnp.minimum(pos, float(w))
        outs.append(mean - uf[:, :, g])
    d = jnp.stack(outs, axis=2).astype(u.dtype)
    y = jnp.einsum('bsgi,gij->bsgj', d, pool_w) + pool_b
    return y.reshape(B, S, POOL_WIDTH) * pool_scale


def _lru_combine(left, right):
    a1, b1 = left
    a2, b2 = right
    return a1 * a2, a2 * b1 + b2


def rg_lru_mixer(xb, gate, conv_w, conv_b, gate_a_w, gate_a_b, gate_x_w, gate_x_b, lru_lambda):
    B, S, _ = xb.shape
    xp = jnp.pad(xb, ((0, 0), (CONV_WIDTH - 1, 0), (0, 0)))
    xc = conv_b + xp[:, 0:S] * conv_w[0]
    for k in range(1, CONV_WIDTH):
        xc = xc + xp[:, k:k + S] * conv_w[k]
    xh = xc.reshape(B, S, LRU_HEADS, LRU_HEAD_DIM)
    r = jax.nn.sigmoid(jnp.einsum('bshi,hij->bshj', xh, gate_a_w) + gate_a_b).reshape(B, S, LRU_WIDTH)
    i = jax.nn.sigmoid(jnp.einsum('bshi,hij->bshj', xh, gate_x_w) + gate_x_b).reshape(B, S, LRU_WIDTH)
    log_a = (LRU_C * r.astype(jnp.float32)) * jax.nn.log_sigmoid(lru_lambda.astype(jnp.float32))
    a = jnp.exp(log_a)
    mult = jnp.sqrt(-jnp.expm1(2.0 * log_a))
    b = mult * (i * xc).astype(jnp.float32)
    _, h = lax.associative_scan(_lru_combine, (a, b), axis=1)
    return h.astype(xb.dtype) * jax.nn.gelu(gate)


def peer_ffn(z, peer_wq, keys1, keys2, peer_u, peer_v):
    B, S, D = z.shape
    zt = z.reshape(-1, PEER_CHUNK, D)
    k1 = keys1.astype(jnp.float32)
    k2 = keys2.astype(jnp.float32)

    def chunk(zc):
        q = (zc @ peer_wq).astype(jnp.float32).reshape(PEER_CHUNK, PEER_HEADS, 2, PEER_HALF)
        s1 = jnp.einsum('chd,hkd->chk', q[:, :, 0], k1)
        s2 = jnp.einsum('chd,hkd->chk', q[:, :, 1], k2)
        v1, i1 = lax.top_k(s1, PEER_TOPK)
        v2, i2 = lax.top_k(s2, PEER_TOPK)
        cand = (v1[..., :, None] + v2[..., None, :]).reshape(PEER_CHUNK, PEER_HEADS, PEER_TOPK * PEER_TOPK)
        cand_idx = (i1[..., :, None] * PEER_N_KEYS + i2[..., None, :]).reshape(PEER_CHUNK, PEER_HEADS, PEER_TOPK * PEER_TOPK)
        top_s, top_pos = lax.top_k(cand, PEER_TOPK)
        expert = jnp.take_along_axis(cand_idx, top_pos, axis=-1)
        gate = jax.nn.softmax(top_s, axis=-1)
        u = jnp.take(peer_u, expert, axis=0)
        act = jax.nn.gelu(jnp.einsum('chkd,cd->chk', u, zc))
        v = jnp.take(peer_v, expert, axis=0)
        w = (gate * act.astype(jnp.float32)).astype(zc.dtype)
        return jnp.einsum('chk,chkd->cd', w, v).astype(zc.dtype)

    return lax.map(chunk, zt).reshape(B, S, D)


def setup_inputs(seed: int = 0) -> dict:
    key = jax.random.key(seed)
    ks = jax.random.split(key, 24)
    L = DEPTH
    f32 = jnp.float32

    def nrm(k, shape, scale):
        return jax.random.normal(k, shape, f32) * scale

    a0 = jax.random.uniform(ks[12], (L, LRU_WIDTH), f32, 0.9, 0.999) ** (1.0 / LRU_C)
    lru_lambda = jnp.log(a0) - jnp.log1p(-a0)
    return {
        "x": nrm(ks[0], (BATCH, SEQ, D_MODEL), 1.0),
        "norm1_g": 1.0 + nrm(ks[1], (L, D_MODEL), 0.02),
        "w_in": nrm(ks[2], (L, D_MODEL, IN_PROJ_WIDTH), D_MODEL ** -0.5),
        "pool_w": nrm(ks[3], (L, len(POOL_WINDOWS), POOL_GROUP, POOL_GROUP), POOL_GROUP ** -0.5),
        "pool_b": nrm(ks[4], (L, len(POOL_WINDOWS), POOL_GROUP), 0.01),
        "pool_scale": 1.0 + nrm(ks[5], (L, POOL_WIDTH), 0.02),
        "conv_w": nrm(ks[6], (L, CONV_WIDTH, LRU_WIDTH), CONV_WIDTH ** -0.5),
        "conv_b": nrm(ks[7], (L, LRU_WIDTH), 0.01),
        "gate_a_w": nrm(ks[8], (L, LRU_HEADS, LRU_HEAD_DIM, LRU_HEAD_DIM), LRU_HEAD_DIM ** -0.5),
        "gate_a_b": nrm(ks[9], (L, LRU_HEADS, LRU_HEAD_DIM), 0.01),
        "gate_x_w": nrm(ks[10], (L, LRU_HEADS, LRU_HEAD_DIM, LRU_HEAD_DIM), LRU_HEAD_DIM ** -0.5),
        "gate_x_b": nrm(ks[11], (L, LRU_HEADS, LRU_HEAD_DIM), 0.01),
        "lru_lambda": lru_lambda,
        "w_out": nrm(ks[13], (L, MIX_WIDTH, D_MODEL), MIX_WIDTH ** -0.5),
        "norm2_g": 1.0 + nrm(ks[14], (L, D_MODEL), 0.02),
        "peer_wq": nrm(ks[15], (L, D_MODEL, PEER_HEADS * PEER_D_KEY), D_MODEL ** -0.5),
        "peer_keys1": nrm(ks[16], (L, PEER_HEADS, PEER_N_KEYS, PEER_HALF), PEER_HALF ** -0.5),
        "peer_keys2": nrm(ks[17], (L, PEER_HEADS, PEER_N_KEYS, PEER_HALF), PEER_HALF ** -0.5),
        "peer_u": nrm(ks[18], (L, PEER_N_EXPERTS, D_MODEL), D_MODEL ** -0.5),
        "peer_v": nrm(ks[19], (L, PEER_N_EXPERTS, D_MODEL), PEER_HEADS ** -0.5),
        "norm_f_g": 1.0 + nrm(ks[20], (D_MODEL,), 0.02),
    }


def reference(x, norm1_g, w_in, pool_w, pool_b, pool_scale, conv_w, conv_b, gate_a_w, gate_a_b,
              gate_x_w, gate_x_b, lru_lambda, w_out, norm2_g, peer_wq, peer_keys1, peer_keys2,
              peer_u, peer_v, norm_f_g):
    h = x
    for l in range(DEPTH):
        z = rmsnorm(h, norm1_g[l])
        proj = z @ w_in[l]
        u_pool = proj[..., :POOL_WIDTH]
        x_lru = proj[..., POOL_WIDTH:POOL_WIDTH + LRU_WIDTH]
        g_lru = proj[..., POOL_WIDTH + LRU_WIDTH:]
        y_pool = pool_mixer(u_pool, pool_w[l], pool_b[l], pool_scale[l])
        y_lru = rg_lru_mixer(x_lru, g_lru, conv_w[l], conv_b[l], gate_a_w[l], gate_a_b[l],
                             gate_x_w[l], gate_x_b[l], lru_lambda[l])
        y = jnp.concatenate([y_pool, y_lru], axis=-1)
        h = h + y @ w_out[l]
        h = h + peer_ffn(rmsnorm(h, norm2_g[l]), peer_wq[l], peer_keys1[l], peer_keys2[l],
                         peer_u[l], peer_v[l])
    return rmsnorm(h, norm_f_g)
```

```python
import contextlib
import numpy as np
import concourse.bass as bass
import concourse.mybir as mybir
from concourse.bass_utils import run_bass_kernel_spmd

F32 = mybir.dt.float32
BF16 = mybir.dt.bfloat16
U32 = mybir.dt.uint32
AF = mybir.ActivationFunctionType
ALU = mybir.AluOpType
AX = mybir.AxisListType

N_DMA_SEMS = 12
D = 2048
NTOK = 4096
NEXP = 16384
EPS = 1e-6
THR = -1e-5

DEBUG = False


class Prog:
    ENGS = ("pe", "act", "dve", "pool", "sp")

    def __init__(self, nc):
        self.nc = nc
        self.ops = []
        self.state = {}
        self.last_op = {}
        self.dmas_since_barrier = []
        self.pending = {}

    def _add(self, eng, fn, reads, writes, is_dma):
        idx = len(self.ops)
        deps = set()
        for k in reads:
            st = self.state.setdefault(k, {"w": {}, "r": {}})
            deps.update(st["w"].values())
        for k in writes:
            st = self.state.setdefault(k, {"w": {}, "r": {}})
            deps.update(st["w"].values())
            deps.update(st["r"].values())
        tag = (eng, idx) if is_dma else (eng,)
        for k in reads:
            self.state[k]["r"][tag] = idx
        for k in writes:
            st = self.state[k]
            st["w"] = {tag: idx}
            st["r"] = {}
        deps.discard(idx)
        deps |= self.pending.pop(eng, set())
        self.ops.append([eng, fn, deps, is_dma])
        if is_dma:
            self.dmas_since_barrier.append(idx)
        else:
            self.last_op[eng] = idx
        return idx

    def I(self, eng, meth, *args, r=(), w=(), dma=False, **kw):
        fn = lambda e: getattr(e, meth)(*args, **kw)
        return self._add(eng, fn, list(r), list(w), dma)

    def barrier(self):
        deps = set(self.last_op.values()) | set(self.dmas_since_barrier)
        for e in self.ENGS:
            self.pending[e] = set(deps) | self.pending.get(e, set())
        self.dmas_since_barrier = []
        self.state = {}

    def emit(self):
        nc = self.nc
        ops = self.ops
        needed = set()
        for i, (eng, fn, deps, is_dma) in enumerate(ops):
            nd = set()
            for d in deps:
                deng, _, _, d_dma = ops[d]
                if deng == eng and not d_dma and not is_dma and eng == "pe":
                    continue
                nd.add(d)
            ops[i][2] = nd
            needed.update(nd)
        with contextlib.ExitStack() as es:
            csem = {e: es.enter_context(nc.semaphore("c_" + e)) for e in self.ENGS}
            dsems = {e: [es.enter_context(nc.semaphore("d_%s_%d" % (e, j))) for j in range(N_DMA_SEMS)]
                     for e in ("sp", "pool")}
            ccount = {e: 0 for e in self.ENGS}
            dma_n = {e: 0 for e in dsems}
            dcount = {e: [0] * N_DMA_SEMS for e in dsems}
            sig = {}
            prev_dma = {}
            for i, (eng, fn, deps, is_dma) in enumerate(ops):
                if is_dma:
                    j = dma_n[eng] % N_DMA_SEMS
                    dma_n[eng] += 1
                    if dcount[eng][j] > 0:
                        prev_dma[i] = (dsems[eng][j], dcount[eng][j])
                    dcount[eng][j] += 16
                    sig[i] = (dsems[eng][j], dcount[eng][j])
                elif i in needed:
                    ccount[eng] += 1
                    sig[i] = (csem[eng], ccount[eng])
            per_eng = {e: [] for e in self.ENGS}
            for i, o in enumerate(ops):
                per_eng[o[0]].append(i)
            handles = {"pe": "tensor", "act": "scalar", "dve": "vector", "pool": "gpsimd", "sp": "sync"}
            self.n_waits = 0

            def make_section(ename):
                def section(e):
                    waited = {}
                    for i in per_eng[ename]:
                        eng, fn, deps, is_dma = ops[i]
                        wl = {}
                        for d in deps:
                            s, v = sig[d]
                            if wl.get(s.num, (None, 0))[1] < v:
                                wl[s.num] = (s, v)
                        if i in prev_dma:
                            s, v = prev_dma[i]
                            if wl.get(s.num, (None, 0))[1] < v:
                                wl[s.num] = (s, v)
                        for snum, (s, v) in wl.items():
                            if waited.get(snum, 0) < v:
                                e.wait_ge(s, v)
                                waited[snum] = v
                                self.n_waits += 1
                        ins = fn(e)
                        if i in sig:
                            ins.then_inc(sig[i][0], 16 if is_dma else 1)
                    if ename in dsems:
                        for j in range(N_DMA_SEMS):
                            if dcount[ename][j] > 0:
                                e.wait_ge(dsems[ename][j], dcount[ename][j])
                return section

            with nc.Block() as block:
                for ename in self.ENGS:
                    if per_eng[ename]:
                        getattr(block, handles[ename])(make_section(ename))
        return nc


class Ring:
    def __init__(self, es, nc, name, n, shape, dt, psum=False):
        alloc = nc.psum_tensor if psum else nc.sbuf_tensor
        self.t = [es.enter_context(alloc("%s%d" % (name, i), shape, dt)) for i in range(n)]
        self.name = name
        self.n = n
        self.i = -1

    def next(self):
        self.i += 1
        s = self.i % self.n
        return self.t[s], (self.name, s)

    def cur(self):
        s = self.i % self.n
        return self.t[s], (self.name, s)


O_G1, O_G2, O_PB, O_PSC, O_CW, O_CB, O_GAB, O_GXB, O_LAM, O_FLAG = 0, 16, 32, 40, 48, 80, 88, 96, 104, 112


def build(phases=("T", "A1", "A2", "B1a", "B1b", "B2"), dbg=()):
    nc = bass.Bass("TRN2", target_bir_lowering=False)

    def din(name, shape, dt=F32):
        return nc.dram_tensor(name, shape, dt, kind="ExternalInput").ap()

    xo = din("xo", [NTOK, D])
    xp = din("xp", [NTOK, D])
    smallw_d = din("smallw", [128, 128])
    gf_d = din("gf", [128, D])
    invdiv_d = din("invdiv", [128, 4, 16])
    w_in = din("w_in", [D, 3072])
    pool_w = din("pool_w", [4, 256, 256])
    gate_a_w = din("gate_a_w", [8, 128, 128])
    gate_x_w = din("gate_x_w", [8, 128, 128])
    w_out = din("w_out", [D, D])
    peer_wq = din("peer_wq", [D, D])
    keys1 = din("keys1", [8, 128, 128])
    keys2 = din("keys2", [8, 128, 128])
    peer_u = din("peer_u", [NEXP, D])
    peer_v = din("peer_v", [NEXP, D])
    out_d = nc.dram_tensor("out", [NTOK, D], F32, kind="ExternalOutput").ap()

    def scr(name, shape, dt):
        return nc.dram_tensor(name, shape, dt, kind=("ExternalOutput" if name in dbg else "Internal")).ap()

    UT = scr("UT", [128, 128, 2048], BF16)
    Vb = scr("Vb", [NEXP, D], BF16)
    YT = scr("YT", [8, 128, 16 * 512], BF16)
    Hd = scr("Hd", [NTOK, D], F32)
    Z2T = scr("Z2T", [8, 128, 16 * 512], BF16)
    Sd = scr("Sd", [NTOK, 2048], F32)
    Wd = scr("Wd", [32, 128, 128 * 128], BF16)
    Vd = scr("Vd", [NTOK, 256], F32)
    Id = scr("Id", [NTOK, 128], F32)

    P = Prog(nc)
    I = P.I

    with contextlib.ExitStack() as gs:
        def sb(es, name, shape, dt=F32):
            return es.enter_context(nc.sbuf_tensor(name, shape, dt))

        def ps(es, name, shape, dt=F32):
            return es.enter_context(nc.psum_tensor(name, shape, dt))

        ident_f = sb(gs, "ident_f", [128, 128])
        ident_b = sb(gs, "ident_b", [128, 128], BF16)
        iota_c = sb(gs, "iota_c", [128, 128])
        smallw = sb(gs, "smallw_s", [128, 128])
        cj = sb(gs, "cj", [128, 8])
        I("sp", "dma_start", out=smallw[:], in_=smallw_d[:, :], w=["smallw"], dma=True)
        I("pool", "iota", iota_c[:], [[1, 128]], base=0, channel_multiplier=-1,
          allow_small_or_imprecise_dtypes=True, w=["iota_c"])
        I("dve", "tensor_single_scalar", ident_f[:], iota_c[:], 0.0, ALU.is_equal, r=["iota_c"], w=["ident_f"])
        I("dve", "tensor_copy", ident_b[:], ident_f[:], r=["ident_f"], w=["ident_b"])
        I("pool", "iota", iota_c[:], [[1, 128]], base=0, channel_multiplier=0,
          allow_small_or_imprecise_dtypes=True, r=["iota_c"], w=["iota_c"])
        iota_cb = sb(gs, "iota_cb", [128, 128], BF16)
        I("dve", "tensor_copy", iota_cb[:], iota_c[:], r=["iota_c"], w=["iota_cb"])
        I("act", "activation", cj[:], smallw[:, O_LAM:O_LAM + 8], AF.Exp, scale=-1.0, r=["smallw"], w=["cj"])
        I("act", "activation", cj[:], cj[:], AF.Ln, bias=1.0, r=["cj"], w=["cj"])
        I("act", "mul", cj[:], cj[:], -8.0, r=["cj"], w=["cj"])
        hcon = sb(gs, "hcon", [128, 24])
        I("act", "mul", hcon[:, 0:16], smallw[:, O_GAB:O_GAB + 16], 0.5, r=["smallw"], w=["hcon"])
        I("act", "mul", hcon[:, 16:24], cj[:], 0.5, r=["cj", "hcon"], w=["hcon"])
        g1b = smallw[:, O_G1:O_G1 + 16].unsqueeze(2).to_broadcast([128, 16, 128])
        g2b = smallw[:, O_G2:O_G2 + 16].unsqueeze(2).to_broadcast([128, 16, 128])

        def rms_and_transpose(es_bufs, src_ap, src_key, dst, dst_key, col0, gb, tag, gcol=None):
            xn_r, ss_r, pTr_r = es_bufs
            xn, kxn = xn_r.next()
            ss, kss = ss_r.next()
            I("act", "activation", xn[:], src_ap, AF.Square, accum_out=ss[:, 0:1], r=[src_key], w=[kxn, kss])
            if gcol is None:
                I("act", "activation", ss[:, 1:2], ss[:, 0:1], AF.Sqrt, scale=1.0 / D, bias=EPS, r=[kss], w=[kss])
                I("dve", "reciprocal", ss[:, 1:2], ss[:, 1:2], r=[kss], w=[kss])
            else:
                I("act", "activation", ss[:, 1:2], ss[:, 0:1], AF.Ln, scale=1.0 / D, bias=EPS, r=[kss], w=[kss])
                I("act", "activation", ss[:, 1:2], ss[:, 1:2], AF.Exp, scale=-0.5, r=[kss], w=[kss])
            I("act", "activation", xn[:], src_ap, AF.Copy, scale=ss[:, 1:2], r=[src_key, kss], w=[kxn])
            pTr, kp = pTr_r.next()
            for dc in range(16):
                I("pe", "transpose", pTr[:, dc, :], xn[:, dc * 128:(dc + 1) * 128], ident_b[:],
                  r=[kxn, "ident_b"], w=[kp])
            if gcol is None:
                I("dve", "tensor_tensor", dst[:, :, col0:col0 + 128], pTr[:], gb, ALU.mult,
                  r=[kp, "smallw"], w=[dst_key])
            else:
                for dc in range(16):
                    I("act", "activation", dst[:, dc, col0:col0 + 128], pTr[:, dc, :], AF.Copy,
                      scale=smallw[:, gcol + dc:gcol + dc + 1], r=[kp, "smallw"], w=[dst_key])
            return ss

        if "T" in phases:
            with contextlib.ExitStack() as es:
                ust = Ring(es, nc, "ust", 2, [128, D], F32)
                vst = Ring(es, nc, "vst", 2, [128, D], F32)
                uts = Ring(es, nc, "uts", 2, [128, D], BF16)
                vbs = Ring(es, nc, "vbs", 2, [128, D], BF16)
                pT = Ring(es, nc, "pT", 2, [128, D], F32, psum=True)
                for c in range(128):
                    u, ku = ust.next()
                    I("sp", "dma_start", out=u[:], in_=peer_u[c * 128:(c + 1) * 128, :], w=[ku], dma=True)
                    p, kp = pT.next()
                    for dc in range(16):
                        I("pe", "transpose", p[:, dc * 128:(dc + 1) * 128], u[:, dc * 128:(dc + 1) * 128],
                          ident_f[:], r=[ku, "ident_f"], w=[kp])
                    o, ko = uts.next()
                    I("act", "copy", o[:, 0:1024], p[:, 0:1024], r=[kp], w=[(ko, 0)])
                    I("dve", "tensor_copy", o[:, 1024:2048], p[:, 1024:2048], r=[kp], w=[(ko, 1)])
                    I("pool", "dma_start", out=UT[c], in_=o[:], r=[(ko, 0), (ko, 1)], dma=True)
            P.barrier()

        if "A1" in phases:
            with contextlib.ExitStack() as es:
                W_in_b = sb(es, "W_in_b", [128, 16, 3072], BF16)
                xs = Ring(es, nc, "xs", 2, [128, D], F32)
                xn_r = Ring(es, nc, "xn", 1, [128, D], BF16)
                ss_r = Ring(es, nc, "ss", 4, [128, 2], F32)
                pTr_r = Ring(es, nc, "pTr", 1, [128, 16, 128], BF16, psum=True)
                zT_r = Ring(es, nc, "zT", 2, [128, 16, 512], BF16)
                pw_b = sb(es, "pw_b", [128, 8, 256], BF16)
                ga_b = sb(es, "ga_b", [128, 8, 128], BF16)
                gx_b = sb(es, "gx_b", [128, 8, 128], BF16)
                invdiv0 = sb(es, "invdiv0", [128, 4, 16])
                tmpd = sb(es, "tmpd", [128, 16])
                for dc in range(16):
                    for hf in range(2):
                        st, kst = xs.next()
                        I("sp", "dma_start", out=st[:, 0:1536],
                          in_=w_in[dc * 128:(dc + 1) * 128, hf * 1536:(hf + 1) * 1536], w=[kst], dma=True)
                        eng = "dve" if hf == 0 else "act"
                        if eng == "dve":
                            I("dve", "tensor_copy", W_in_b[:, dc, hf * 1536:(hf + 1) * 1536], st[:, 0:1536],
                              r=[kst], w=[("W_in", dc, hf)])
                        else:
                            I("act", "copy", W_in_b[:, dc, hf * 1536:(hf + 1) * 1536], st[:, 0:1536],
                              r=[kst], w=[("W_in", dc, hf)])
                st, kst = xs.next()
                I("sp", "dma_start", out=st[:, 0:2048].rearrange("p (a j) -> p a j", a=8),
                  in_=pool_w.rearrange("g (c p) j -> p (g c) j", p=128), w=[kst], dma=True)
                I("dve", "tensor_copy", pw_b[:], st[:, 0:2048].rearrange("p (a j) -> p a j", a=8), r=[kst], w=["pw_b"])
                st, kst = xs.next()
                I("sp", "dma_start", out=st[:, 0:1024].rearrange("p (a j) -> p a j", a=8),
                  in_=gate_a_w.rearrange("h i j -> i h j"), w=[kst], dma=True)
                I("dve", "tensor_copy", ga_b[:], st[:, 0:1024].rearrange("p (a j) -> p a j", a=8), r=[kst], w=["ga_b"])
                st, kst = xs.next()
                I("sp", "dma_start", out=st[:, 0:1024].rearrange("p (a j) -> p a j", a=8),
                  in_=gate_x_w.rearrange("h i j -> i h j"), w=[kst], dma=True)
                I("dve", "tensor_copy", gx_b[:], st[:, 0:1024].rearrange("p (a j) -> p a j", a=8), r=[kst], w=["gx_b"])
                I("sp", "dma_start", out=invdiv0[:], in_=invdiv_d[:, :, :], w=["invdiv0"], dma=True)
                W_in_keys = [("W_in", dc, hf) for dc in range(16) for hf in range(2)]

                pj = Ring(es, nc, "pj", 3, [128, 512], F32, psum=True)
                pg = Ring(es, nc, "pg", 2, [128, 512], F32, psum=True)
                pl = Ring(es, nc, "pl", 1, [128, 512], F32, psum=True)
                xl_r = Ring(es, nc, "xl", 2, [128, 515], F32)
                up_r = Ring(es, nc, "up", 4, [128, 528], F32)
                xhalo = sb(es, "xhalo", [128, 8, 3])
                uhalo = sb(es, "uhalo", [128, 8, 16])
                state = sb(es, "state", [128, 8])
                xc_r = Ring(es, nc, "xc", 2, [128, 512], F32)
                xcb_r = Ring(es, nc, "xcb", 1, [128, 512], BF16)
                r_r = Ring(es, nc, "rr", 2, [128, 512], F32)
                ig_r = Ring(es, nc, "ig", 2, [128, 512], F32)
                a_r = Ring(es, nc, "aa", 2, [128, 512], F32)
                hs_r = Ring(es, nc, "hs", 2, [128, 512], F32)
                gg_r = Ring(es, nc, "gg", 2, [128, 512], BF16)
                sA = sb(es, "sA", [128, 528])
                sB = sb(es, "sB", [128, 528])
                db_r = Ring(es, nc, "db", 4, [128, 512], BF16)
                yc_r = Ring(es, nc, "yc", 2, [128, 512], BF16)
                halfc = sb(es, "halfc", [128, 512])
                I("pool", "memset", halfc[:], 0.5, w=["halfc"])
                I("dve", "memset", xhalo[:], 0.0, w=[("xhalo", j) for j in range(8)])
                I("dve", "memset", uhalo[:], 0.0, w=[("uhalo", j) for j in range(8)])
                I("dve", "memset", state[:], 0.0, w=[("state", j) for j in range(8)])

                zcur = [None, None]

                def proj(cc):
                    p, kp = pj.next()
                    for dc in range(16):
                        I("pe", "matmul", p[:], W_in_b[:, dc, cc * 128:(cc + 1) * 128], zcur[0][:, dc, :],
                          start=(dc == 0), stop=(dc == 15), r=W_in_keys + [zcur[1]], w=[kp])
                    return p, kp

                def rms_tile(ti_):
                    src_ = xp if ti_ < 8 else xo
                    t0_ = (ti_ % 8) * 512
                    z, kz = zT_r.next()
                    for sub in range(4):
                        x_, kx = xs.next()
                        I("sp", "dma_start", out=x_[:], in_=src_[t0_ + sub * 128:t0_ + (sub + 1) * 128, :], w=[kx], dma=True)
                        rms_and_transpose((xn_r, ss_r, pTr_r), x_[:], kx, z, kz, sub * 128, g1b, "a1")
                    return z, kz

                def store_y(ti, ch, y, ky):
                    I("pool", "dma_start", out=YT[ti - 8][:, ch * 512:(ch + 1) * 512], in_=y[:], r=[ky], dma=True)

                znext = rms_tile(0)
                for ti in range(16):
                    prefix = ti < 8
                    zcur[0], zcur[1] = znext
                    def stage_x(j):
                        p, kp = proj(8 + j)
                        xl, kxl = xl_r.next()
                        I("dve", "tensor_copy", xl[:, 0:3], xhalo[:, j, :], r=[("xhalo", j)], w=[(kxl, 0)])
                        I("act", "copy", xl[:, 3:515], p[:], r=[kp], w=[(kxl, 1)])
                        return xl, [(kxl, 0), (kxl, 1)]

                    def stageA(j, xl, kxl):
                        d = {}
                        if not prefix:
                            p2, kp2 = proj(16 + j)
                            gg, kgg = gg_r.next()
                            I("act", "activation", gg[:], p2[:], AF.Gelu_apprx_tanh, r=[kp2], w=[kgg])
                            d["gg"], d["kgg"] = gg, kgg
                        xc, kxc = xc_r.next()
                        cw = lambda k: smallw[:, O_CW + k * 8 + j:O_CW + k * 8 + j + 1]
                        I("dve", "tensor_scalar", xc[:], xl[:, 0:512], cw(0), smallw[:, O_CB + j:O_CB + j + 1],
                          ALU.mult, ALU.add, r=kxl + ["smallw"], w=[kxc])
                        for k in range(1, 4):
                            I("dve", "scalar_tensor_tensor", xc[:], xl[:, k:k + 512], cw(k), xc[:], ALU.mult, ALU.add,
                              r=kxl + [kxc, "smallw"], w=[kxc])
                        I("dve", "tensor_copy", xhalo[:, j, :], xl[:, 512:515], r=kxl, w=[("xhalo", j)])
                        xcb, kxcb = xcb_r.next()
                        I("act", "copy", xcb[:], xc[:], r=[kxc], w=[kxcb])
                        pr, kpr = pg.next()
                        I("pe", "matmul", pr[:], ga_b[:, j, :], xcb[:], start=True, stop=True, r=["ga_b", kxcb], w=[kpr])
                        pi, kpi = pg.next()
                        I("pe", "matmul", pi[:], gx_b[:, j, :], xcb[:], start=True, stop=True, r=["gx_b", kxcb], w=[kpi])
                        rr, krr = r_r.next()
                        I("act", "activation", rr[:], pr[:], AF.Tanh, scale=0.5, bias=hcon[:, j:j + 1],
                          r=[kpr, "hcon"], w=[krr])
                        ig, kig = ig_r.next()
                        I("act", "activation", ig[:], pi[:], AF.Tanh, scale=0.5, bias=hcon[:, 8 + j:9 + j],
                          r=[kpi, "hcon"], w=[kig])
                        aa, kaa = a_r.next()
                        I("act", "activation", aa[:], rr[:], AF.Exp, scale=hcon[:, 16 + j:17 + j], bias=hcon[:, 16 + j:17 + j],
                          r=[krr, "hcon"], w=[kaa])
                        mm, kmm = rr, krr
                        I("pool", "tensor_tensor", mm[:], aa[:], aa[:], ALU.mult, r=[kaa], w=[kmm])
                        I("pool", "tensor_scalar", mm[:], mm[:], -1.0, 1.0, ALU.mult, ALU.add, r=[kmm], w=[kmm])
                        I("pool", "tensor_tensor", mm[:], mm[:], halfc[:], ALU.pow, r=[kmm, "halfc"], w=[kmm])
                        d.update(xc=xc, kxc=kxc, ig=ig, kig=kig, aa=aa, kaa=kaa, mm=mm, kmm=kmm, j=j)
                        return d

                    def stageB(d):
                        j = d["j"]
                        ig, kig = d["ig"], d["kig"]
                        I("dve", "scalar_tensor_tensor", ig[:], ig[:], 1.0, d["xc"][:], ALU.add, ALU.mult,
                          r=[kig, d["kxc"]], w=[kig])
                        I("dve", "scalar_tensor_tensor", ig[:], ig[:], 0.5, d["mm"][:], ALU.mult, ALU.mult,
                          r=[kig, d["kmm"]], w=[kig])
                        hs, khs = hs_r.next()
                        I("dve", "tensor_tensor_scan", hs[:], d["aa"][:], ig[:], state[:, j:j + 1], ALU.mult, ALU.add,
                          r=[d["kaa"], kig, ("state", j)], w=[khs])
                        if ti == 7:
                            I("dve", "tensor_tensor", state[:, j:j + 1], hs[:, 511:512],
                              smallw[:, O_FLAG:O_FLAG + 1], ALU.mult, r=[khs, "smallw"], w=[("state", j)])
                        else:
                            I("dve", "tensor_copy", state[:, j:j + 1], hs[:, 511:512], r=[khs], w=[("state", j)])
                        if not prefix:
                            y, ky = yc_r.next()
                            I("dve", "tensor_tensor", y[:], hs[:], d["gg"][:], ALU.mult, r=[khs, d["kgg"]], w=[ky])
                            store_y(ti, 8 + j, y, ky)

                    nxt = stage_x(0)
                    dprev = None
                    for j in range(9):
                        dcur = None
                        if j < 8:
                            xl, kxl = nxt
                            nxt = stage_x(j + 1) if j < 7 else None
                            dcur = stageA(j, xl, kxl)
                        if dprev is not None:
                            stageB(dprev)
                        dprev = dcur
                    zkeep = (zcur[0], zcur[1])
                    if ti < 15:
                        znext = rms_tile(ti + 1)
                    zcur[0], zcur[1] = zkeep
                    if ti >= 7:
                        def stage_u(ch):
                            p, kp = proj(ch)
                            up, kup = up_r.next()
                            I("dve", "tensor_copy", up[:, 0:16], uhalo[:, ch, :], r=[("uhalo", ch)], w=[(kup, 0)])
                            I("act", "copy", up[:, 16:528], p[:], r=[kp], w=[(kup, 1)])
                            return up, [(kup, 0), (kup, 1)]

                        nxt = [stage_u(0), stage_u(1)]
                        for g in range(4):
                            ups = nxt
                            nxt = [stage_u(2 * g + 2), stage_u(2 * g + 3)] if g < 3 else None
                            for ic in range(2):
                                I("dve", "tensor_copy", uhalo[:, 2 * g + ic, :], ups[ic][0][:, 512:528], r=ups[ic][1],
                                  w=[("uhalo", 2 * g + ic)])
                            if prefix:
                                continue
                            dbs = []
                            for ic in range(2):
                                up, kup = ups[ic]
                                cur, kcur = up, kup
                                bufs = [(sA, "sA"), (sB, "sB")]
                                sh = 1
                                for lvl in range(g + 1):
                                    nb, knb = bufs[lvl % 2]
                                    I("pool", "tensor_tensor", nb[:, sh:528], cur[:, sh:528], cur[:, 0:528 - sh], ALU.add,
                                      r=kcur, w=[knb])
                                    cur, kcur = nb, [knb]
                                    sh *= 2
                                w = float(2 ** (g + 1))
                                d_, kd = db_r.next()
                                I("dve", "scalar_tensor_tensor", d_[:], cur[:, 16:528], 1.0 / w, up[:, 16:528],
                                  ALU.mult, ALU.subtract, r=kcur + kup, w=[kd])
                                if ti == 8:
                                    I("dve", "tensor_tensor", tmpd[:], cur[:, 16:32], invdiv0[:, g, :], ALU.mult,
                                      r=kcur + ["invdiv0"], w=["tmpd"])
                                    I("dve", "tensor_tensor", d_[:, 0:16], tmpd[:], up[:, 16:32], ALU.subtract,
                                      r=["tmpd"] + kup, w=[kd])
                                dbs.append((d_, kd))
                            for jo in range(2):
                                ch = g * 2 + jo
                                pp, kpp = pl.next()
                                for ic in range(2):
                                    I("pe", "matmul", pp[:], pw_b[:, g * 2 + ic, jo * 128:(jo + 1) * 128], dbs[ic][0][:],
                                      start=(ic == 0), stop=(ic == 1), r=["pw_b", dbs[ic][1]], w=[kpp])
                                y, ky = yc_r.next()
                                I("dve", "tensor_scalar", y[:], pp[:], smallw[:, O_PB + ch:O_PB + ch + 1],
                                  smallw[:, O_PSC + ch:O_PSC + ch + 1], ALU.add, ALU.mult, r=[kpp, "smallw"], w=[ky])
                                store_y(ti, ch, y, ky)
            P.barrier()

        if "A2" in phases:
            with contextlib.ExitStack() as es:
                W_out_b = sb(es, "W_out_b", [128, 16, D], BF16)
                xs = Ring(es, nc, "xs2", 2, [128, D], F32)
                yT_r = Ring(es, nc, "yTr", 2, [128, 16, 512], BF16)
                hs_r = Ring(es, nc, "hsub", 2, [128, D], F32)
                po = Ring(es, nc, "po", 4, [128, 512], F32, psum=True)
                for mc in range(16):
                    st, kst = xs.next()
                    I("sp", "dma_start", out=st[:], in_=w_out[mc * 128:(mc + 1) * 128, :], w=[kst], dma=True)
                    if mc % 2 == 0:
                        I("dve", "tensor_copy", W_out_b[:, mc, :], st[:], r=[kst], w=[("W_out", mc)])
                    else:
                        I("act", "copy", W_out_b[:, mc, :], st[:], r=[kst], w=[("W_out", mc)])
                W_out_keys = [("W_out", mc) for mc in range(16)]
                for ti in range(8):
                    yT, kyT = yT_r.next()
                    I("sp", "dma_start", out=yT[:].rearrange("p a t -> p (a t)"), in_=YT[ti], w=[kyT], dma=True)
                    for sub in range(4):
                        r0 = ti * 512 + sub * 128
                        x_, kx = xs.next()
                        I("sp", "dma_start", out=x_[:], in_=xo[r0:r0 + 128, :], w=[kx], dma=True)
                        h_, kh = hs_r.next()
                        for cb in range(4):
                            p, kp = po.next()
                            for mc in range(16):
                                I("pe", "matmul", p[:], yT[:, mc, sub * 128:(sub + 1) * 128],
                                  W_out_b[:, mc, cb * 512:(cb + 1) * 512], start=(mc == 0), stop=(mc == 15),
                                  r=[kyT] + W_out_keys, w=[kp])
                            I("dve", "tensor_tensor", h_[:, cb * 512:(cb + 1) * 512], p[:], x_[:, cb * 512:(cb + 1) * 512],
                              ALU.add, r=[kp, kx], w=[(kh, cb)])
                        I("pool", "dma_start", out=Hd[r0:r0 + 128, :], in_=h_[:], r=[(kh, cb) for cb in range(4)], dma=True)
            P.barrier()

        if "B1a" in phases:
            with contextlib.ExitStack() as es:
                Wq_b = sb(es, "Wq_b", [128, 16, D], BF16)
                KT = sb(es, "KT", [128, 16, 128], BF16)
                hin = Ring(es, nc, "hin", 2, [128, D], F32)
                xn_r = Ring(es, nc, "xn2", 2, [128, D], BF16)
                ss_r = Ring(es, nc, "ss2", 4, [128, 2], F32)
                pTr_r = Ring(es, nc, "pTr2", 1, [128, 16, 128], BF16, psum=True)
                z2T_r = Ring(es, nc, "z2T", 2, [128, 16, 512], BF16)
                qT = sb(es, "qT", [128, 16, 512], BF16)
                pq = Ring(es, nc, "pq", 2, [128, 512], F32, psum=True)
                psc = Ring(es, nc, "psc", 1, [128, 2048], F32, psum=True)
                S_r = Ring(es, nc, "Ss", 5, [128, 2048], F32)
                Swk_r = Ring(es, nc, "Swk", 2, [128, 128], F32)
                v_r = Ring(es, nc, "v1", 5, [128, 16, 16], F32)
                idx_r = Ring(es, nc, "idx1", 5, [128, 8, 16], U32)
                idxf_r = Ring(es, nc, "idxf1", 5, [128, 128], F32)
                for mc in range(16):
                    st, kst = hin.next()
                    I("sp", "dma_start", out=st[:], in_=peer_wq[mc * 128:(mc + 1) * 128, :], w=[kst], dma=True)
                    if mc % 2 == 0:
                        I("dve", "tensor_copy", Wq_b[:, mc, :], st[:], r=[kst], w=[("Wq", mc)])
                    else:
                        I("act", "copy", Wq_b[:, mc, :], st[:], r=[kst], w=[("Wq", mc)])
                Wq_keys = [("Wq", mc) for mc in range(16)]
                for half, kd in ((0, keys1), (1, keys2)):
                    st, kst = hin.next()
                    I("sp", "dma_start", out=st[:, 0:1024].rearrange("p (h d) -> p h d", h=8),
                      in_=kd.rearrange("h i d -> i h d"), w=[kst], dma=True)
                    p, kp = psc.next()
                    for h in range(8):
                        I("pe", "transpose", p[:, h * 128:(h + 1) * 128], st[:, h * 128:(h + 1) * 128], ident_f[:],
                          r=[kst, "ident_f"], w=[kp])
                    I("dve", "tensor_copy", KT[:].rearrange("p (h f) i -> p h f i", f=2)[:, :, half, :],
                      p[:, 0:1024].rearrange("p (h i) -> p h i", h=8), r=[kp], w=[("KT", half)])
                for grp in range(8):
                    z2T, kz = z2T_r.next()
                    for sub in range(4):
                        r0 = grp * 512 + sub * 128
                        h_, kh = hin.next()
                        I("sp", "dma_start", out=h_[:], in_=Hd[r0:r0 + 128, :], w=[kh], dma=True)
                        rms_and_transpose((xn_r, ss_r, pTr_r), h_[:], kh, z2T, kz, sub * 128, g2b, "b1", gcol=O_G2)
                    I("pool", "dma_start", out=Z2T[grp], in_=z2T[:].rearrange("p a t -> p (a t)"), r=[kz], dma=True)
                    for m in range(16):
                        p, kp = pq.next()
                        for dc in range(16):
                            I("pe", "matmul", p[:], Wq_b[:, dc, m * 128:(m + 1) * 128], z2T[:, dc, :],
                              start=(dc == 0), stop=(dc == 15), r=Wq_keys + [kz], w=[kp])
                        I("act", "copy", qT[:, m, :], p[:], r=[kp], w=[("qT", m)])
                    for sub in range(4):
                        r0 = grp * 512 + sub * 128
                        p, kp = psc.next()
                        for m in range(16):
                            I("pe", "matmul", p[:, m * 128:(m + 1) * 128], qT[:, m, sub * 128:(sub + 1) * 128], KT[:, m, :],
                              start=True, stop=True, r=[("qT", m), ("KT", 0), ("KT", 1)], w=[kp])
                        S, kS = S_r.next()
                        I("act", "copy", S[:, 0:1024], p[:, 0:1024], r=[kp], w=[(kS, 0)])
                        I("act", "copy", S[:, 1024:2048], p[:, 1024:2048], r=[kp], w=[(kS, 1)])
                        I("pool", "dma_start", out=Sd[r0:r0 + 128, :], in_=S[:], r=[(kS, 0), (kS, 1)], dma=True)
                        Sk = [(kS, 0), (kS, 1)]
                        S3 = S[:].rearrange("p (m i) -> p m i", m=16)
                        v, kv_ = v_r.next()
                        ix, kix = idx_r.next()
                        for m in range(16):
                            sw_, ksw = Swk_r.next()
                            I("dve", "max", out=v[:, m, 0:8], in_=S3[:, m, :], r=Sk, w=[kv_])
                            I("dve", "match_replace", out=sw_[:], in_to_replace=v[:, m, 0:8], in_values=S3[:, m, :],
                              imm_value=-1e30, r=Sk + [kv_], w=[ksw])
                            I("dve", "max", out=v[:, m, 8:16], in_=sw_[:], r=[ksw], w=[kv_])
                            if m % 2 == 0:
                                h = m // 2
                                I("dve", "max_index", out=ix[:, h, 0:8], in_max=v[:, m, 0:8], in_values=S3[:, m, :],
                                  r=Sk + [kv_], w=[kix])
                                I("dve", "max_index", out=ix[:, h, 8:16], in_max=v[:, m, 8:16], in_values=sw_[:],
                                  r=[ksw, kv_], w=[kix])
                        ixf, kixf = idxf_r.next()
                        I("dve", "tensor_copy", ixf[:], ix[:].rearrange("p h k -> p (h k)"), r=[kix], w=[kixf])
                        I("pool", "dma_start", out=Vd[r0:r0 + 128, :], in_=v[:].rearrange("p m k -> p (m k)"), r=[kv_], dma=True)
                        I("pool", "dma_start", out=Id[r0:r0 + 128, :], in_=ixf[:], r=[kixf], dma=True)
            P.barrier()

        if "B1b" in phases:
            with contextlib.ExitStack() as es:
                S2_r = Ring(es, nc, "S2b", 2, [128, 8, 128], F32)
                v_r = Ring(es, nc, "vb", 2, [128, 16, 16], F32)
                idxf_r = Ring(es, nc, "idxfb", 2, [128, 128], F32)
                idxT_r = Ring(es, nc, "idxT", 2, [128, 128], BF16)
                cand = sb(es, "cand", [128, 8, 256])
                cwk = sb(es, "cwk", [128, 8, 256])
                top = sb(es, "top", [128, 8, 16])
                tmp16 = sb(es, "tmp16", [128, 8, 16])
                zz_r = Ring(es, nc, "zz", 2, [128, 8], F32)
                c1_r = Ring(es, nc, "c1", 2, [128, 8, 16], F32)
                Xg_r = Ring(es, nc, "Xg", 2, [128, 128, 16], F32)
                Eg_r = Ring(es, nc, "Eg", 2, [128, 128, 16], BF16)
                Y = sb(es, "Yt", [128, 128, 128], BF16)
                YTs = sb(es, "YTs", [128, 128, 128], BF16)
                R_r = Ring(es, nc, "Rr", 2, [128, 128, 64], BF16)
                WT_r = Ring(es, nc, "WTs", 2, [128, 64, 128], BF16)
                pY = Ring(es, nc, "pY", 2, [128, 128, 4], F32, psum=True)
                pI = Ring(es, nc, "pI", 1, [128, 128], F32, psum=True)
                pW = Ring(es, nc, "pW", 4, [128, 64, 8], F32, psum=True)
                Ykeys = [("Y", h) for h in range(8)]
                YTkeys = [("YTs", ig) for ig in range(32)]

                def front(tt):
                    r0 = tt * 128
                    S2, kS = S2_r.next()
                    I("sp", "dma_start", out=S2[:],
                      in_=Sd[r0:r0 + 128, :].rearrange("p (h f i) -> p h f i", f=2, i=128)[:, :, 1, :], w=[kS], dma=True)
                    v, kv_ = v_r.next()
                    I("sp", "dma_start", out=v[:].rearrange("p m k -> p (m k)"), in_=Vd[r0:r0 + 128, :], w=[kv_], dma=True)
                    ixf, kixf = idxf_r.next()
                    I("sp", "dma_start", out=ixf[:], in_=Id[r0:r0 + 128, :], w=[kixf], dma=True)
                    v4 = v[:].rearrange("p (h f) k -> p h f k", f=2)
                    I("dve", "tensor_tensor", cand[:].rearrange("p h (a b) -> p h a b", a=16),
                      v4[:, :, 0, :].unsqueeze(3).to_broadcast([128, 8, 16, 16]),
                      v4[:, :, 1, :].unsqueeze(2).to_broadcast([128, 8, 16, 16]), ALU.add, r=[kv_], w=["cand"])
                    for h in range(8):
                        I("dve", "max", out=top[:, h, 0:8], in_=cand[:, h, :], r=["cand"], w=["top"])
                        I("dve", "match_replace", out=cwk[:, h, :], in_to_replace=top[:, h, 0:8], in_values=cand[:, h, :],
                          imm_value=-1e30, r=["cand", "top"], w=["cwk"])
                        I("dve", "max", out=top[:, h, 8:16], in_=cwk[:, h, :], r=["cwk"], w=["top"])
                    tau_b = top[:, :, 15:16].to_broadcast([128, 8, 16])
                    zz, kzz = zz_r.next()
                    c1, kc1 = c1_r.next()
                    I("dve", "tensor_tensor", tmp16[:], top[:], tau_b, ALU.subtract, r=["top"], w=["tmp16"])
                    I("dve", "tensor_tensor", c1[:], v4[:, :, 0, :], tau_b, ALU.subtract, r=[kv_, "top"], w=[kc1])
                    I("act", "activation", tmp16[:], tmp16[:], AF.Exp, r=["tmp16"], w=["tmp16"])
                    I("dve", "tensor_reduce", zz[:], tmp16[:], AX.X, ALU.add, r=["tmp16"], w=[kzz])
                    I("act", "activation", zz[:], zz[:], AF.Ln, r=[kzz], w=[kzz])
                    I("act", "mul", zz[:], zz[:], -1.0, r=[kzz], w=[kzz])
                    pi_, kpi = pI.next()
                    I("pe", "transpose", pi_[:], ixf[:], ident_f[:], r=[kixf, "ident_f"], w=[kpi])
                    idxT, kiT = idxT_r.next()
                    I("act", "copy", idxT[:], pi_[:], r=[kpi], w=[kiT])
                    return dict(S2=S2, kS=kS, zz=zz, kzz=kzz, c1=c1, kc1=kc1, idxT=idxT, kiT=kiT, tt=tt)

                def ybuild(f, heads=range(8)):
                    for h in heads:
                        Xg, kX = Xg_r.next()
                        I("pool", "tensor_tensor", Xg[:],
                          f["S2"][:, h, :].unsqueeze(2).to_broadcast([128, 128, 16]),
                          f["c1"][:, h, :].unsqueeze(1).to_broadcast([128, 128, 16]), ALU.add,
                          r=[f["kS"], f["kc1"]], w=[kX])
                        Eg, kE = Eg_r.next()
                        I("act", "activation", Eg[:], Xg[:], AF.Exp, bias=f["zz"][:, h:h + 1], r=[kX, f["kzz"]], w=[kE])
                        I("dve", "scalar_tensor_tensor", Y[:, :, h * 16:(h + 1) * 16], Xg[:], THR, Eg[:],
                          ALU.is_ge, ALU.mult, r=[kX, kE], w=[("Y", h)])

                def trans():
                    for ig in range(32):
                        p, kp = pY.next()
                        for ii in range(4):
                            i2 = ig * 4 + ii
                            I("pe", "matmul", p[:, :, ii], Y[:, i2, :], ident_b[:], start=True, stop=True,
                              r=Ykeys + ["ident_b"], w=[kp])
                        I("act", "copy", YTs[:, :, ig * 4:(ig + 1) * 4], p[:], r=[kp], w=[("YTs", ig)])

                def rbuild(f, ch):
                    R, kR = R_r.next()
                    I("dve", "tensor_tensor", R[:],
                      iota_cb[:, ch * 64:(ch + 1) * 64].unsqueeze(1).to_broadcast([128, 128, 64]),
                      f["idxT"][:].unsqueeze(2).to_broadcast([128, 128, 64]), ALU.is_equal,
                      r=["iota_cb", f["kiT"]], w=[kR])
                    return R, kR

                def mm(f, ch, R, kR, WT, kWT, tgs):
                    for tg in tgs:
                        p, kp = pW.next()
                        for ti_ in range(8):
                            t = tg * 8 + ti_
                            I("pe", "matmul", p[:, :, ti_], YTs[:, t, :], R[:, t, :], start=True, stop=True,
                              r=YTkeys + [kR], w=[kp])
                        I("act", "copy", WT[:, :, tg * 8:(tg + 1) * 8], p[:], r=[kp], w=[(kWT, tg)])

                def wstore(f, ch, WT, kWT):
                    I("sp", "dma_start", out=Wd[f["tt"]][:, ch * 8192:(ch + 1) * 8192],
                      in_=WT[:].rearrange("p c t -> p (c t)"), r=[(kWT, tg) for tg in range(16)], dma=True)

                f_cur = front(0)
                ybuild(f_cur)
                for tt in range(32):
                    trans()
                    f_next = front(tt + 1) if tt < 31 else None
                    Rs = [rbuild(f_cur, 0), None]
                    WTs = [WT_r.next(), WT_r.next()]
                    for h in range(8):
                        ch = h // 4
                        if h == 3:
                            Rs[1] = rbuild(f_cur, 1)
                        if f_next is not None:
                            ybuild(f_next, [h])
                        q = h % 4
                        mm(f_cur, ch, Rs[ch][0], Rs[ch][1], WTs[ch][0], WTs[ch][1], range(q * 4, q * 4 + 4))
                        if q == 3:
                            wstore(f_cur, ch, WTs[ch][0], WTs[ch][1])
                    f_cur = f_next
            P.barrier()

        if "B2" in phases:
            with contextlib.ExitStack() as es:
                gf = sb(es, "gf_s", [128, D])
                I("sp", "dma_start", out=gf[:], in_=gf_d[:, :], w=["gf"], dma=True)
                z2T_r = Ring(es, nc, "z2Tb", 2, [128, 16, 512], BF16)
                vst_r = Ring(es, nc, "vstb", 3, [128, D], F32)
                hacc = sb(es, "hacc", [128, 4, D])
                NB = 4
                ut_r = Ring(es, nc, "utb", 2, [128, NB, 2048], BF16)
                vb_r = Ring(es, nc, "vbb", 2, [128, NB, 2048], BF16)
                wb_r = Ring(es, nc, "wbb", 2, [128, 4, NB, 128], BF16)
                gl_r = Ring(es, nc, "gl", 2, [128, 512], BF16)
                pt_r = Ring(es, nc, "pt", 2, [128, NB, 512], BF16)
                pa = Ring(es, nc, "pa", 2, [128, 512], F32, psum=True)
                po = Ring(es, nc, "po2", 1, [128, 4, 512], F32, psum=True)
                ss_r = Ring(es, nc, "ss3", 4, [128, 2], F32)
                junk = sb(es, "junk", [128, D], BF16)
                ob_r = Ring(es, nc, "ob", 2, [128, D], F32)
                znx = z2T_r.next()
                I("sp", "dma_start", out=znx[0][:].rearrange("p a t -> p (a t)"), in_=Z2T[0], w=[znx[1]], dma=True)
                for grp in range(8):
                    z2T, kz2 = znx
                    if grp < 7:
                        znx = z2T_r.next()
                        I("sp", "dma_start", out=znx[0][:].rearrange("p a t -> p (a t)"), in_=Z2T[grp + 1], w=[znx[1]], dma=True)
                    nblk = 128 // NB
                    prev = None
                    for b in range(nblk + 1):
                        cur = None
                        if b < nblk:
                            c0 = b * NB
                            ut, kut = ut_r.next()
                            I("sp", "dma_start", out=ut[:], in_=UT[c0:c0 + NB].rearrange("c p f -> p c f"), w=[kut], dma=True)
                            vb, kvb = vb_r.next()
                            wb, kwb = wb_r.next()
                            I("sp", "dma_start", out=wb[:].rearrange("p s c t -> p s (c t)"),
                              in_=Wd[grp * 4:(grp + 1) * 4, :, c0 * 128:(c0 + NB) * 128].rearrange("s p f -> p s f"),
                              w=[kwb], dma=True)
                            pt, kpt = pt_r.next()
                            cur = (vb, kvb, pt, kpt)
                        for step in range(4):
                            if b < nblk:
                                c = step
                                p, kp = pa.next()
                                for dc in range(16):
                                    I("pe", "matmul", p[:], ut[:, c, dc * 128:(dc + 1) * 128], z2T[:, dc, :],
                                      start=(dc == 0), stop=(dc == 15), r=[kut, kz2], w=[kp])
                                gl, kgl = gl_r.next()
                                I("act", "activation", gl[:], p[:], AF.Gelu_apprx_tanh, r=[kp], w=[kgl])
                                vs_, kvs = vst_r.next()
                                I("sp", "dma_start", out=vs_[:], in_=peer_v[(c0 + c) * 128:(c0 + c + 1) * 128, :],
                                  w=[kvs], dma=True)
                                I("act", "copy", vb[:, c, :], vs_[:], r=[kvs], w=[(kvb, c)])
                                I("pool", "tensor_tensor", pt[:, c, :].rearrange("p (s t) -> p s t", s=4),
                                  gl[:].rearrange("p (s t) -> p s t", s=4), wb[:, :, c, :], ALU.mult,
                                  r=[kgl, kwb], w=[(kpt, c)])
                            if prev is not None:
                                pvb, pkvb, ppt, pkpt = prev
                                s = step
                                o, ko = po.next()
                                for c in range(NB):
                                    for cb in range(4):
                                        I("pe", "matmul", o[:, cb, :], ppt[:, c, s * 128:(s + 1) * 128],
                                          pvb[:, c, cb * 512:(cb + 1) * 512], start=(c == 0), stop=(c == NB - 1),
                                          r=[(pkpt, c), (pkvb, c)], w=[ko])
                                I("dve", "tensor_tensor", hacc[:, s, :], hacc[:, s, :], o[:].rearrange("p a b -> p (a b)"),
                                  ALU.add, r=[ko, ("hacc", s)], w=[("hacc", s)])
                        prev = cur
                        if b == 0:
                            I("sp", "dma_start", out=hacc[:],
                              in_=Hd[grp * 512:(grp + 1) * 512, :].rearrange("(s p) d -> p s d", p=128),
                              w=[("hacc", s) for s in range(4)], dma=True)
                    for s in range(4):
                        ss, kss = ss_r.next()
                        I("act", "activation", junk[:], hacc[:, s, :], AF.Square, accum_out=ss[:, 0:1],
                          r=[("hacc", s)], w=["junk", kss])
                        I("act", "activation", ss[:, 1:2], ss[:, 0:1], AF.Sqrt, scale=1.0 / D, bias=EPS, r=[kss], w=[kss])
                        I("dve", "reciprocal", ss[:, 1:2], ss[:, 1:2], r=[kss], w=[kss])
                        ob, kob = ob_r.next()
                        I("dve", "scalar_tensor_tensor", ob[:], hacc[:, s, :], ss[:, 1:2], gf[:], ALU.mult, ALU.mult,
                          r=[("hacc", s), kss, "gf"], w=[kob])
                        r0 = grp * 512 + s * 128
                        I("pool", "dma_start", out=out_d[r0:r0 + 128, :], in_=ob[:], r=[kob], dma=True)
        P.emit()
    return nc, P


def _chunkT(v):
    return np.ascontiguousarray(np.asarray(v, np.float32).reshape(-1, 128).T)


def make_in_maps(inp):
    x = np.asarray(inp["x"], np.float32)
    sw = np.zeros((128, 128), np.float32)
    sw[:, O_G1:O_G1 + 16] = _chunkT(inp["norm1_g"][0])
    sw[:, O_G2:O_G2 + 16] = _chunkT(inp["norm2_g"][0])
    sw[:, O_PB:O_PB + 8] = _chunkT(inp["pool_b"][0].reshape(-1))
    sw[:, O_PSC:O_PSC + 8] = _chunkT(inp["pool_scale"][0])
    for k in range(4):
        sw[:, O_CW + k * 8:O_CW + (k + 1) * 8] = _chunkT(inp["conv_w"][0][k])
    sw[:, O_CB:O_CB + 8] = _chunkT(inp["conv_b"][0])
    sw[:, O_GAB:O_GAB + 8] = _chunkT(inp["gate_a_b"][0].reshape(-1))
    sw[:, O_GXB:O_GXB + 8] = _chunkT(inp["gate_x_b"][0].reshape(-1))
    sw[:, O_LAM:O_LAM + 8] = _chunkT(inp["lru_lambda"][0])
    gf = np.ascontiguousarray(np.broadcast_to(np.asarray(inp["norm_f_g"], np.float32)[None, :], (128, D)))
    shared = {
        "gf": gf,
        "w_in": np.ascontiguousarray(inp["w_in"][0], dtype=np.float32),
        "pool_w": np.ascontiguousarray(inp["pool_w"][0], dtype=np.float32),
        "gate_a_w": np.ascontiguousarray(inp["gate_a_w"][0], dtype=np.float32),
        "gate_x_w": np.ascontiguousarray(inp["gate_x_w"][0], dtype=np.float32),
        "w_out": np.ascontiguousarray(inp["w_out"][0], dtype=np.float32),
        "peer_wq": np.ascontiguousarray(inp["peer_wq"][0], dtype=np.float32),
        "keys1": np.ascontiguousarray(inp["peer_keys1"][0], dtype=np.float32),
        "keys2": np.ascontiguousarray(inp["peer_keys2"][0], dtype=np.float32),
        "peer_u": np.ascontiguousarray(inp["peer_u"][0], dtype=np.float32),
        "peer_v": np.ascontiguousarray(inp["peer_v"][0], dtype=np.float32),
    }
    zeros = np.zeros((NTOK, D), np.float32)
    maps = []
    for core in range(8):
        b, half = core // 2, core % 2
        m = dict(shared)
        m["xo"] = np.ascontiguousarray(x[b, half * NTOK:(half + 1) * NTOK])
        m["xp"] = np.ascontiguousarray(x[b, 0:NTOK]) if half == 1 else zeros
        s = sw.copy()
        s[:, O_FLAG] = float(half)
        m["smallw"] = s
        pos = half * NTOK + np.arange(16, dtype=np.float32) + 1.0
        inv = np.stack([1.0 / np.minimum(pos, float(w)) for w in (2, 4, 8, 16)], 0).astype(np.float32)
        m["invdiv"] = np.ascontiguousarray(np.broadcast_to(inv[None], (128, 4, 16)))
        maps.append(m)
    return maps


def kernel(**inputs):
    nc, _ = build()
    maps = make_in_maps(inputs)
    res = run_bass_kernel_spmd(nc, maps, core_ids=list(range(8)))
    out = np.zeros((4, 8192, D), np.float32)
    for core in range(8):
        b, half = core // 2, core % 2
        out[b, half * NTOK:(half + 1) * NTOK] = np.asarray(res.results[core]["out"], np.float32)
    return out
```

```python
import contextlib
import numpy as np
import concourse.bass as bass
import concourse.mybir as mybir
from concourse.bass_utils import run_bass_kernel_spmd

F32 = mybir.dt.float32
BF16 = mybir.dt.bfloat16
U32 = mybir.dt.uint32
AF = mybir.ActivationFunctionType
ALU = mybir.AluOpType
AX = mybir.AxisListType

N_DMA_SEMS = 12
D = 2048
NTOK = 4096
NEXP = 16384
EPS = 1e-6
THR = -1e-5

DEBUG = False


class Prog:
    ENGS = ("pe", "act", "dve", "pool", "sp")

    def __init__(self, nc):
        self.nc = nc
        self.ops = []
        self.state = {}
        self.last_op = {}
        self.dmas_since_barrier = []
        self.pending = {}

    def _add(self, eng, fn, reads, writes, is_dma):
        idx = len(self.ops)
        deps = set()
        for k in reads:
            st = self.state.setdefault(k, {"w": {}, "r": {}})
            deps.update(st["w"].values())
        for k in writes:
            st = self.state.setdefault(k, {"w": {}, "r": {}})
            deps.update(st["w"].values())
            deps.update(st["r"].values())
        tag = (eng, idx) if is_dma else (eng,)
        for k in reads:
            self.state[k]["r"][tag] = idx
        for k in writes:
            st = self.state[k]
            st["w"] = {tag: idx}
            st["r"] = {}
        deps.discard(idx)
        deps |= self.pending.pop(eng, set())
        self.ops.append([eng, fn, deps, is_dma])
        if is_dma:
            self.dmas_since_barrier.append(idx)
        else:
            self.last_op[eng] = idx
        return idx

    def I(self, eng, meth, *args, r=(), w=(), dma=False, **kw):
        fn = lambda e: getattr(e, meth)(*args, **kw)
        return self._add(eng, fn, list(r), list(w), dma)

    def barrier(self):
        deps = set(self.last_op.values()) | set(self.dmas_since_barrier)
        for e in self.ENGS:
            self.pending[e] = set(deps) | self.pending.get(e, set())
        self.dmas_since_barrier = []
        self.state = {}

    def emit(self):
        nc = self.nc
        ops = self.ops
        needed = set()
        for i, (eng, fn, deps, is_dma) in enumerate(ops):
            nd = set()
            for d in deps:
                deng, _, _, d_dma = ops[d]
                if deng == eng and not d_dma and not is_dma and eng == "pe":
                    continue
                nd.add(d)
            ops[i][2] = nd
            needed.update(nd)
        with contextlib.ExitStack() as es:
            csem = {e: es.enter_context(nc.semaphore("c_" + e)) for e in self.ENGS}
            dsems = {e: [es.enter_context(nc.semaphore("d_%s_%d" % (e, j))) for j in range(N_DMA_SEMS)]
                     for e in ("sp", "pool")}
            ccount = {e: 0 for e in self.ENGS}
            dma_n = {e: 0 for e in dsems}
            dcount = {e: [0] * N_DMA_SEMS for e in dsems}
            sig = {}
            prev_dma = {}
            for i, (eng, fn, deps, is_dma) in enumerate(ops):
                if is_dma:
                    j = dma_n[eng] % N_DMA_SEMS
                    dma_n[eng] += 1
                    if dcount[eng][j] > 0:
                        prev_dma[i] = (dsems[eng][j], dcount[eng][j])
                    dcount[eng][j] += 16
                    sig[i] = (dsems[eng][j], dcount[eng][j])
                elif i in needed:
                    ccount[eng] += 1
                    sig[i] = (csem[eng], ccount[eng])
            per_eng = {e: [] for e in self.ENGS}
            for i, o in enumerate(ops):
                per_eng[o[0]].append(i)
            handles = {"pe": "tensor", "act": "scalar", "dve": "vector", "pool": "gpsimd", "sp": "sync"}
            self.n_waits = 0

            def make_section(ename):
                def section(e):
                    waited = {}
                    for i in per_eng[ename]:
                        eng, fn, deps, is_dma = ops[i]
                        wl = {}
                        for d in deps:
                            s, v = sig[d]
                            if wl.get(s.num, (None, 0))[1] < v:
                                wl[s.num] = (s, v)
                        if i in prev_dma:
                            s, v = prev_dma[i]
                            if wl.get(s.num, (None, 0))[1] < v:
                                wl[s.num] = (s, v)
                        for snum, (s, v) in wl.items():
                            if waited.get(snum, 0) < v:
                                e.wait_ge(s, v)
                                waited[snum] = v
                                self.n_waits += 1
                        ins = fn(e)
                        if i in sig:
                            ins.then_inc(sig[i][0], 16 if is_dma else 1)
                    if ename in dsems:
                        for j in range(N_DMA_SEMS):
                            if dcount[ename][j] > 0:
                                e.wait_ge(dsems[ename][j], dcount[ename][j])
                return section

            with nc.Block() as block:
                for ename in self.ENGS:
                    if per_eng[ename]:
                        getattr(block, handles[ename])(make_section(ename))
        return nc


class Ring:
    def __init__(self, es, nc, name, n, shape, dt, psum=False):
        alloc = nc.psum_tensor if psum else nc.sbuf_tensor
        self.t = [es.enter_context(alloc("%s%d" % (name, i), shape, dt)) for i in range(n)]
        self.name = name
        self.n = n
        self.i = -1

    def next(self):
        self.i += 1
        s = self.i % self.n
        return self.t[s], (self.name, s)

    def cur(self):
        s = self.i % self.n
        return self.t[s], (self.name, s)


O_G1, O_G2, O_PB, O_PSC, O_CW, O_CB, O_GAB, O_GXB, O_LAM, O_FLAG = 0, 16, 32, 40, 48, 80, 88, 96, 104, 112


def build(phases=("T", "A1", "A2", "B1a", "B1b", "B2"), dbg=()):
    nc = bass.Bass("TRN2", target_bir_lowering=False)

    def din(name, shape, dt=F32):
        return nc.dram_tensor(name, shape, dt, kind="ExternalInput").ap()

    xo = din("xo", [NTOK, D])
    xp = din("xp", [NTOK, D])
    smallw_d = din("smallw", [128, 128])
    gf_d = din("gf", [128, D])
    invdiv_d = din("invdiv", [128, 4, 16])
    w_in = din("w_in", [D, 3072])
    pool_w = din("pool_w", [4, 256, 256])
    gate_a_w = din("gate_a_w", [8, 128, 128])
    gate_x_w = din("gate_x_w", [8, 128, 128])
    w_out = din("w_out", [D, D])
    peer_wq = din("peer_wq", [D, D])
    keys1 = din("keys1", [8, 128, 128])
    keys2 = din("keys2", [8, 128, 128])
    peer_u = din("peer_u", [NEXP, D])
    peer_v = din("peer_v", [NEXP, D])
    out_d = nc.dram_tensor("out", [NTOK, D], F32, kind="ExternalOutput").ap()

    def scr(name, shape, dt):
        return nc.dram_tensor(name, shape, dt, kind=("ExternalOutput" if name in dbg else "Internal")).ap()

    UT = scr("UT", [128, 128, 2048], BF16)
    Vb = scr("Vb", [NEXP, D], BF16)
    YT = scr("YT", [8, 128, 16 * 512], BF16)
    Hd = scr("Hd", [NTOK, D], F32)
    Z2T = scr("Z2T", [8, 128, 16 * 512], BF16)
    Sd = scr("Sd", [NTOK, 2048], F32)
    Wd = scr("Wd", [32, 128, 128 * 128], BF16)
    Vd = scr("Vd", [NTOK, 256], F32)
    Id = scr("Id", [NTOK, 128], F32)

    P = Prog(nc)
    I = P.I

    with contextlib.ExitStack() as gs:
        def sb(es, name, shape, dt=F32):
            return es.enter_context(nc.sbuf_tensor(name, shape, dt))

        def ps(es, name, shape, dt=F32):
            return es.enter_context(nc.psum_tensor(name, shape, dt))

        ident_f = sb(gs, "ident_f", [128, 128])
        ident_b = sb(gs, "ident_b", [128, 128], BF16)
        iota_c = sb(gs, "iota_c", [128, 128])
        smallw = sb(gs, "smallw_s", [128, 128])
        cj = sb(gs, "cj", [128, 8])
        I("sp", "dma_start", out=smallw[:], in_=smallw_d[:, :], w=["smallw"], dma=True)
        I("pool", "iota", iota_c[:], [[1, 128]], base=0, channel_multiplier=-1,
          allow_small_or_imprecise_dtypes=True, w=["iota_c"])
        I("dve", "tensor_single_scalar", ident_f[:], iota_c[:], 0.0, ALU.is_equal, r=["iota_c"], w=["ident_f"])
        I("dve", "tensor_copy", ident_b[:], ident_f[:], r=["ident_f"], w=["ident_b"])
        I("pool", "iota", iota_c[:], [[1, 128]], base=0, channel_multiplier=0,
          allow_small_or_imprecise_dtypes=True, r=["iota_c"], w=["iota_c"])
        iota_cb = sb(gs, "iota_cb", [128, 128], BF16)
        I("dve", "tensor_copy", iota_cb[:], iota_c[:], r=["iota_c"], w=["iota_cb"])
        I("act", "activation", cj[:], smallw[:, O_LAM:O_LAM + 8], AF.Exp, scale=-1.0, r=["smallw"], w=["cj"])
        I("act", "activation", cj[:], cj[:], AF.Ln, bias=1.0, r=["cj"], w=["cj"])
        I("act", "mul", cj[:], cj[:], -8.0, r=["cj"], w=["cj"])
        hcon = sb(gs, "hcon", [128, 24])
        I("act", "mul", hcon[:, 0:16], smallw[:, O_GAB:O_GAB + 16], 0.5, r=["smallw"], w=["hcon"])
        I("act", "mul", hcon[:, 16:24], cj[:], 0.5, r=["cj", "hcon"], w=["hcon"])
        g1b = smallw[:, O_G1:O_G1 + 16].unsqueeze(2).to_broadcast([128, 16, 128])
        g2b = smallw[:, O_G2:O_G2 + 16].unsqueeze(2).to_broadcast([128, 16, 128])

        def rms_and_transpose(es_bufs, src_ap, src_key, dst, dst_key, col0, gb, tag, gcol=None):
            xn_r, ss_r, pTr_r = es_bufs
            xn, kxn = xn_r.next()
            ss, kss = ss_r.next()
            I("act", "activation", xn[:], src_ap, AF.Square, accum_out=ss[:, 0:1], r=[src_key], w=[kxn, kss])
            if gcol is None:
                I("act", "activation", ss[:, 1:2], ss[:, 0:1], AF.Sqrt, scale=1.0 / D, bias=EPS, r=[kss], w=[kss])
                I("dve", "reciprocal", ss[:, 1:2], ss[:, 1:2], r=[kss], w=[kss])
            else:
                I("act", "activation", ss[:, 1:2], ss[:, 0:1], AF.Ln, scale=1.0 / D, bias=EPS, r=[kss], w=[kss])
                I("act", "activation", ss[:, 1:2], ss[:, 1:2], AF.Exp, scale=-0.5, r=[kss], w=[kss])
            I("act", "activation", xn[:], src_ap, AF.Copy, scale=ss[:, 1:2], r=[src_key, kss], w=[kxn])
            pTr, kp = pTr_r.next()
            for dc in range(16):
                I("pe", "transpose", pTr[:, dc, :], xn[:, dc * 128:(dc + 1) * 128], ident_b[:],
                  r=[kxn, "ident_b"], w=[kp])
            if gcol is None:
                I("dve", "tensor_tensor", dst[:, :, col0:col0 + 128], pTr[:], gb, ALU.mult,
                  r=[kp, "smallw"], w=[dst_key])
            else:
                for dc in range(16):
                    I("act", "activation", dst[:, dc, col0:col0 + 128], pTr[:, dc, :], AF.Copy,
                      scale=smallw[:, gcol + dc:gcol + dc + 1], r=[kp, "smallw"], w=[dst_key])
            return ss

        if "T" in phases:
            with contextlib.ExitStack() as es:
                ust = Ring(es, nc, "ust", 2, [128, D], F32)
                vst = Ring(es, nc, "vst", 2, [128, D], F32)
                uts = Ring(es, nc, "uts", 2, [128, D], BF16)
                vbs = Ring(es, nc, "vbs", 2, [128, D], BF16)
                pT = Ring(es, nc, "pT", 2, [128, D], F32, psum=True)
                for c in range(128):
                    u, ku = ust.next()
                    I("sp", "dma_start", out=u[:], in_=peer_u[c * 128:(c + 1) * 128, :], w=[ku], dma=True)
                    p, kp = pT.next()
                    for dc in range(16):
                        I("pe", "transpose", p[:, dc * 128:(dc + 1) * 128], u[:, dc * 128:(dc + 1) * 128],
                          ident_f[:], r=[ku, "ident_f"], w=[kp])
                    o, ko = uts.next()
                    I("act", "copy", o[:, 0:1024], p[:, 0:1024], r=[kp], w=[(ko, 0)])
                    I("dve", "tensor_copy", o[:, 1024:2048], p[:, 1024:2048], r=[kp], w=[(ko, 1)])
                    I("pool", "dma_start", out=UT[c], in_=o[:], r=[(ko, 0), (ko, 1)], dma=True)
            P.barrier()

        if "A1" in phases:
            with contextlib.ExitStack() as es:
                W_in_b = sb(es, "W_in_b", [128, 16, 3072], BF16)
                xs = Ring(es, nc, "xs", 2, [128, D], F32)
                xn_r = Ring(es, nc, "xn", 1, [128, D], BF16)
                ss_r = Ring(es, nc, "ss", 4, [128, 2], F32)
                pTr_r = Ring(es, nc, "pTr", 1, [128, 16, 128], BF16, psum=True)
                zT_r = Ring(es, nc, "zT", 2, [128, 16, 512], BF16)
                pw_b = sb(es, "pw_b", [128, 8, 256], BF16)
                ga_b = sb(es, "ga_b", [128, 8, 128], BF16)
                gx_b = sb(es, "gx_b", [128, 8, 128], BF16)
                invdiv0 = sb(es, "invdiv0", [128, 4, 16])
                tmpd = sb(es, "tmpd", [128, 16])
                for dc in range(16):
                    for hf in range(2):
                        st, kst = xs.next()
                        I("sp", "dma_start", out=st[:, 0:1536],
                          in_=w_in[dc * 128:(dc + 1) * 128, hf * 1536:(hf + 1) * 1536], w=[kst], dma=True)
                        eng = "dve" if hf == 0 else "act"
                        if eng == "dve":
                            I("dve", "tensor_copy", W_in_b[:, dc, hf * 1536:(hf + 1) * 1536], st[:, 0:1536],
                              r=[kst], w=[("W_in", dc, hf)])
                        else:
                            I("act", "copy", W_in_b[:, dc, hf * 1536:(hf + 1) * 1536], st[:, 0:1536],
                              r=[kst], w=[("W_in", dc, hf)])
                st, kst = xs.next()
                I("sp", "dma_start", out=st[:, 0:2048].rearrange("p (a j) -> p a j", a=8),
                  in_=pool_w.rearrange("g (c p) j -> p (g c) j", p=128), w=[kst], dma=True)
                I("dve", "tensor_copy", pw_b[:], st[:, 0:2048].rearrange("p (a j) -> p a j", a=8), r=[kst], w=["pw_b"])
                st, kst = xs.next()
                I("sp", "dma_start", out=st[:, 0:1024].rearrange("p (a j) -> p a j", a=8),
                  in_=gate_a_w.rearrange("h i j -> i h j"), w=[kst], dma=True)
                I("dve", "tensor_copy", ga_b[:], st[:, 0:1024].rearrange("p (a j) -> p a j", a=8), r=[kst], w=["ga_b"])
                st, kst = xs.next()
                I("sp", "dma_start", out=st[:, 0:1024].rearrange("p (a j) -> p a j", a=8),
                  in_=gate_x_w.rearrange("h i j -> i h j"), w=[kst], dma=True)
                I("dve", "tensor_copy", gx_b[:], st[:, 0:1024].rearrange("p (a j) -> p a j", a=8), r=[kst], w=["gx_b"])
                I("sp", "dma_start", out=invdiv0[:], in_=invdiv_d[:, :, :], w=["invdiv0"], dma=True)
                W_in_keys = [("W_in", dc, hf) for dc in range(16) for hf in range(2)]

                pj = Ring(es, nc, "pj", 3, [128, 512], F32, psum=True)
                pg = Ring(es, nc, "pg", 2, [128, 512], F32, psum=True)
                pl = Ring(es, nc, "pl", 1, [128, 512], F32, psum=True)
                xl_r = Ring(es, nc, "xl", 2, [128, 515], F32)
                up_r = Ring(es, nc, "up", 4, [128, 528], F32)
                xhalo = sb(es, "xhalo", [128, 8, 3])
                uhalo = sb(es, "uhalo", [128, 8, 16])
                state = sb(es, "state", [128, 8])
                xc_r = Ring(es, nc, "xc", 2, [128, 512], F32)
                xcb_r = Ring(es, nc, "xcb", 1, [128, 512], BF16)
                r_r = Ring(es, nc, "rr", 2, [128, 512], F32)
                ig_r = Ring(es, nc, "ig", 2, [128, 512], F32)
                a_r = Ring(es, nc, "aa", 2, [128, 512], F32)
                hs_r = Ring(es, nc, "hs", 2, [128, 512], F32)
                gg_r = Ring(es, nc, "gg", 2, [128, 512], BF16)
                sA = sb(es, "sA", [128, 528])
                sB = sb(es, "sB", [128, 528])
                db_r = Ring(es, nc, "db", 4, [128, 512], BF16)
                yc_r = Ring(es, nc, "yc", 2, [128, 512], BF16)
                I("dve", "memset", xhalo[:], 0.0, w=[("xhalo", j) for j in range(8)])
                I("dve", "memset", uhalo[:], 0.0, w=[("uhalo", j) for j in range(8)])
                I("dve", "memset", state[:], 0.0, w=[("state", j) for j in range(8)])

                zcur = [None, None]

                def proj(cc):
                    p, kp = pj.next()
                    for dc in range(16):
                        I("pe", "matmul", p[:], W_in_b[:, dc, cc * 128:(cc + 1) * 128], zcur[0][:, dc, :],
                          start=(dc == 0), stop=(dc == 15), r=W_in_keys + [zcur[1]], w=[kp])
                    return p, kp

                def rms_tile(ti_):
                    src_ = xp if ti_ < 8 else xo
                    t0_ = (ti_ % 8) * 512
                    z, kz = zT_r.next()
                    for sub in range(4):
                        x_, kx = xs.next()
                        I("sp", "dma_start", out=x_[:], in_=src_[t0_ + sub * 128:t0_ + (sub + 1) * 128, :], w=[kx], dma=True)
                        rms_and_transpose((xn_r, ss_r, pTr_r), x_[:], kx, z, kz, sub * 128, g1b, "a1")
                    return z, kz

                def store_y(ti, ch, y, ky):
                    I("pool", "dma_start", out=YT[ti - 8][:, ch * 512:(ch + 1) * 512], in_=y[:], r=[ky], dma=True)

                znext = rms_tile(0)
                for ti in range(16):
                    prefix = ti < 8
                    zcur[0], zcur[1] = znext
                    def stage_x(j):
                        p, kp = proj(8 + j)
                        xl, kxl = xl_r.next()
                        I("dve", "tensor_copy", xl[:, 0:3], xhalo[:, j, :], r=[("xhalo", j)], w=[(kxl, 0)])
                        I("act", "copy", xl[:, 3:515], p[:], r=[kp], w=[(kxl, 1)])
                        return xl, [(kxl, 0), (kxl, 1)]

                    def stageA(j, xl, kxl):
                        d = {}
                        if not prefix:
                            p2, kp2 = proj(16 + j)
                            gg, kgg = gg_r.next()
                            I("act", "activation", gg[:], p2[:], AF.Gelu_apprx_tanh, r=[kp2], w=[kgg])
                            d["gg"], d["kgg"] = gg, kgg
                        xc, kxc = xc_r.next()
                        cw = lambda k: smallw[:, O_CW + k * 8 + j:O_CW + k * 8 + j + 1]
                        I("dve", "tensor_scalar", xc[:], xl[:, 0:512], cw(0), smallw[:, O_CB + j:O_CB + j + 1],
                          ALU.mult, ALU.add, r=kxl + ["smallw"], w=[kxc])
                        for k in range(1, 4):
                            I("dve", "scalar_tensor_tensor", xc[:], xl[:, k:k + 512], cw(k), xc[:], ALU.mult, ALU.add,
                              r=kxl + [kxc, "smallw"], w=[kxc])
                        I("dve", "tensor_copy", xhalo[:, j, :], xl[:, 512:515], r=kxl, w=[("xhalo", j)])
                        xcb, kxcb = xcb_r.next()
                        I("act", "copy", xcb[:], xc[:], r=[kxc], w=[kxcb])
                        pr, kpr = pg.next()
                        I("pe", "matmul", pr[:], ga_b[:, j, :], xcb[:], start=True, stop=True, r=["ga_b", kxcb], w=[kpr])
                        pi, kpi = pg.next()
                        I("pe", "matmul", pi[:], gx_b[:, j, :], xcb[:], start=True, stop=True, r=["gx_b", kxcb], w=[kpi])
                        rr, krr = r_r.next()
                        I("act", "activation", rr[:], pr[:], AF.Tanh, scale=0.5, bias=hcon[:, j:j + 1],
                          r=[kpr, "hcon"], w=[krr])
                        ig, kig = ig_r.next()
                        I("act", "activation", ig[:], pi[:], AF.Tanh, scale=0.5, bias=hcon[:, 8 + j:9 + j],
                          r=[kpi, "hcon"], w=[kig])
                        aa, kaa = a_r.next()
                        I("act", "activation", aa[:], rr[:], AF.Exp, scale=hcon[:, 16 + j:17 + j], bias=hcon[:, 16 + j:17 + j],
                          r=[krr, "hcon"], w=[kaa])
                        mm, kmm = rr, krr
                        I("act", "activation", mm[:], rr[:], AF.Exp, scale=cj[:, j:j + 1], bias=cj[:, j:j + 1],
                          r=[krr, "cj"], w=[kmm])
                        I("act", "activation", mm[:], mm[:], AF.Sqrt, scale=-1.0, bias=1.0, r=[kmm], w=[kmm])
                        d.update(xc=xc, kxc=kxc, ig=ig, kig=kig, aa=aa, kaa=kaa, mm=mm, kmm=kmm, j=j)
                        return d

                    def stageB(d):
                        j = d["j"]
                        ig, kig = d["ig"], d["kig"]
                        I("dve", "scalar_tensor_tensor", ig[:], ig[:], 1.0, d["xc"][:], ALU.add, ALU.mult,
                          r=[kig, d["kxc"]], w=[kig])
                        I("dve", "scalar_tensor_tensor", ig[:], ig[:], 0.5, d["mm"][:], ALU.mult, ALU.mult,
                          r=[kig, d["kmm"]], w=[kig])
                        hs, khs = hs_r.next()
                        I("dve", "tensor_tensor_scan", hs[:], d["aa"][:], ig[:], state[:, j:j + 1], ALU.mult, ALU.add,
                          r=[d["kaa"], kig, ("state", j)], w=[khs])
                        if ti == 7:
                            I("dve", "tensor_tensor", state[:, j:j + 1], hs[:, 511:512],
                              smallw[:, O_FLAG:O_FLAG + 1], ALU.mult, r=[khs, "smallw"], w=[("state", j)])
                        else:
                            I("dve", "tensor_copy", state[:, j:j + 1], hs[:, 511:512], r=[khs], w=[("state", j)])
                        if not prefix:
                            y, ky = yc_r.next()
                            I("dve", "tensor_tensor", y[:], hs[:], d["gg"][:], ALU.mult, r=[khs, d["kgg"]], w=[ky])
                            store_y(ti, 8 + j, y, ky)

                    nxt = stage_x(0)
                    dprev = None
                    for j in range(9):
                        dcur = None
                        if j < 8:
                            xl, kxl = nxt
                            nxt = stage_x(j + 1) if j < 7 else None
                            dcur = stageA(j, xl, kxl)
                        if dprev is not None:
                            stageB(dprev)
                        dprev = dcur
                    zkeep = (zcur[0], zcur[1])
                    if ti < 15:
                        znext = rms_tile(ti + 1)
                    zcur[0], zcur[1] = zkeep
                    if ti >= 7:
                        def stage_u(ch):
                            p, kp = proj(ch)
                            up, kup = up_r.next()
                            I("dve", "tensor_copy", up[:, 0:16], uhalo[:, ch, :], r=[("uhalo", ch)], w=[(kup, 0)])
                            I("act", "copy", up[:, 16:528], p[:], r=[kp], w=[(kup, 1)])
                            return up, [(kup, 0), (kup, 1)]

                        nxt = [stage_u(0), stage_u(1)]
                        for g in range(4):
                            ups = nxt
                            nxt = [stage_u(2 * g + 2), stage_u(2 * g + 3)] if g < 3 else None
                            for ic in range(2):
                                I("dve", "tensor_copy", uhalo[:, 2 * g + ic, :], ups[ic][0][:, 512:528], r=ups[ic][1],
                                  w=[("uhalo", 2 * g + ic)])
                            if prefix:
                                continue
                            dbs = []
                            for ic in range(2):
                                up, kup = ups[ic]
                                cur, kcur = up, kup
                                bufs = [(sA, "sA"), (sB, "sB")]
                                sh = 1
                                for lvl in range(g + 1):
                                    nb, knb = bufs[lvl % 2]
                                    lo = 2 * sh - 1
                                    I("pool", "tensor_tensor", nb[:, lo:528], cur[:, lo:528], cur[:, lo - sh:528 - sh], ALU.add,
                                      r=kcur, w=[knb])
                                    cur, kcur = nb, [knb]
                                    sh *= 2
                                w = float(2 ** (g + 1))
                                d_, kd = db_r.next()
                                I("dve", "scalar_tensor_tensor", d_[:], cur[:, 16:528], 1.0 / w, up[:, 16:528],
                                  ALU.mult, ALU.subtract, r=kcur + kup, w=[kd])
                                if ti == 8:
                                    I("dve", "tensor_tensor", tmpd[:], cur[:, 16:32], invdiv0[:, g, :], ALU.mult,
                                      r=kcur + ["invdiv0"], w=["tmpd"])
                                    I("dve", "tensor_tensor", d_[:, 0:16], tmpd[:], up[:, 16:32], ALU.subtract,
                                      r=["tmpd"] + kup, w=[kd])
                                dbs.append((d_, kd))
                            for jo in range(2):
                                ch = g * 2 + jo
                                pp, kpp = pl.next()
                                for ic in range(2):
                                    I("pe", "matmul", pp[:], pw_b[:, g * 2 + ic, jo * 128:(jo + 1) * 128], dbs[ic][0][:],
                                      start=(ic == 0), stop=(ic == 1), r=["pw_b", dbs[ic][1]], w=[kpp])
                                y, ky = yc_r.next()
                                I("dve", "tensor_scalar", y[:], pp[:], smallw[:, O_PB + ch:O_PB + ch + 1],
                                  smallw[:, O_PSC + ch:O_PSC + ch + 1], ALU.add, ALU.mult, r=[kpp, "smallw"], w=[ky])
                                store_y(ti, ch, y, ky)
            P.barrier()

        if "A2" in phases:
            with contextlib.ExitStack() as es:
                W_out_b = sb(es, "W_out_b", [128, 16, D], BF16)
                xs = Ring(es, nc, "xs2", 2, [128, D], F32)
                yT_r = Ring(es, nc, "yTr", 2, [128, 16, 512], BF16)
                hs_r = Ring(es, nc, "hsub", 2, [128, D], F32)
                po = Ring(es, nc, "po", 4, [128, 512], F32, psum=True)
                for mc in range(16):
                    st, kst = xs.next()
                    I("sp", "dma_start", out=st[:], in_=w_out[mc * 128:(mc + 1) * 128, :], w=[kst], dma=True)
                    if mc % 2 == 0:
                        I("dve", "tensor_copy", W_out_b[:, mc, :], st[:], r=[kst], w=[("W_out", mc)])
                    else:
                        I("act", "copy", W_out_b[:, mc, :], st[:], r=[kst], w=[("W_out", mc)])
                W_out_keys = [("W_out", mc) for mc in range(16)]
                for ti in range(8):
                    yT, kyT = yT_r.next()
                    I("sp", "dma_start", out=yT[:].rearrange("p a t -> p (a t)"), in_=YT[ti], w=[kyT], dma=True)
                    for sub in range(4):
                        r0 = ti * 512 + sub * 128
                        x_, kx = xs.next()
                        I("sp", "dma_start", out=x_[:], in_=xo[r0:r0 + 128, :], w=[kx], dma=True)
                        h_, kh = hs_r.next()
                        for cb in range(4):
                            p, kp = po.next()
                            for mc in range(16):
                                I("pe", "matmul", p[:], yT[:, mc, sub * 128:(sub + 1) * 128],
                                  W_out_b[:, mc, cb * 512:(cb + 1) * 512], start=(mc == 0), stop=(mc == 15),
                                  r=[kyT] + W_out_keys, w=[kp])
                            I("dve", "tensor_tensor", h_[:, cb * 512:(cb + 1) * 512], p[:], x_[:, cb * 512:(cb + 1) * 512],
                              ALU.add, r=[kp, kx], w=[(kh, cb)])
                        I("pool", "dma_start", out=Hd[r0:r0 + 128, :], in_=h_[:], r=[(kh, cb) for cb in range(4)], dma=True)
            P.barrier()

        if "B1a" in phases:
            with contextlib.ExitStack() as es:
                Wq_b = sb(es, "Wq_b", [128, 16, D], BF16)
                KT = sb(es, "KT", [128, 16, 128], BF16)
                hin = Ring(es, nc, "hin", 2, [128, D], F32)
                xn_r = Ring(es, nc, "xn2", 2, [128, D], BF16)
                ss_r = Ring(es, nc, "ss2", 4, [128, 2], F32)
                pTr_r = Ring(es, nc, "pTr2", 1, [128, 16, 128], BF16, psum=True)
                z2T_r = Ring(es, nc, "z2T", 2, [128, 16, 512], BF16)
                qT = sb(es, "qT", [128, 16, 512], BF16)
                pq = Ring(es, nc, "pq", 2, [128, 512], F32, psum=True)
                psc = Ring(es, nc, "psc", 1, [128, 2048], F32, psum=True)
                S_r = Ring(es, nc, "Ss", 5, [128, 2048], F32)
                Swk_r = Ring(es, nc, "Swk", 2, [128, 128], F32)
                v_r = Ring(es, nc, "v1", 5, [128, 16, 16], F32)
                idx_r = Ring(es, nc, "idx1", 5, [128, 8, 16], U32)
                idxf_r = Ring(es, nc, "idxf1", 5, [128, 128], F32)
                for mc in range(16):
                    st, kst = hin.next()
                    I("sp", "dma_start", out=st[:], in_=peer_wq[mc * 128:(mc + 1) * 128, :], w=[kst], dma=True)
                    if mc % 2 == 0:
                        I("dve", "tensor_copy", Wq_b[:, mc, :], st[:], r=[kst], w=[("Wq", mc)])
                    else:
                        I("act", "copy", Wq_b[:, mc, :], st[:], r=[kst], w=[("Wq", mc)])
                Wq_keys = [("Wq", mc) for mc in range(16)]
                for half, kd in ((0, keys1), (1, keys2)):
                    st, kst = hin.next()
                    I("sp", "dma_start", out=st[:, 0:1024].rearrange("p (h d) -> p h d", h=8),
                      in_=kd.rearrange("h i d -> i h d"), w=[kst], dma=True)
                    p, kp = psc.next()
                    for h in range(8):
                        I("pe", "transpose", p[:, h * 128:(h + 1) * 128], st[:, h * 128:(h + 1) * 128], ident_f[:],
                          r=[kst, "ident_f"], w=[kp])
                    I("dve", "tensor_copy", KT[:].rearrange("p (h f) i -> p h f i", f=2)[:, :, half, :],
                      p[:, 0:1024].rearrange("p (h i) -> p h i", h=8), r=[kp], w=[("KT", half)])
                for grp in range(8):
                    z2T, kz = z2T_r.next()
                    for sub in range(4):
                        r0 = grp * 512 + sub * 128
                        h_, kh = hin.next()
                        I("sp", "dma_start", out=h_[:], in_=Hd[r0:r0 + 128, :], w=[kh], dma=True)
                        rms_and_transpose((xn_r, ss_r, pTr_r), h_[:], kh, z2T, kz, sub * 128, g2b, "b1", gcol=O_G2)
                    I("pool", "dma_start", out=Z2T[grp], in_=z2T[:].rearrange("p a t -> p (a t)"), r=[kz], dma=True)
                    for m in range(16):
                        p, kp = pq.next()
                        for dc in range(16):
                            I("pe", "matmul", p[:], Wq_b[:, dc, m * 128:(m + 1) * 128], z2T[:, dc, :],
                              start=(dc == 0), stop=(dc == 15), r=Wq_keys + [kz], w=[kp])
                        I("act", "copy", qT[:, m, :], p[:], r=[kp], w=[("qT", m)])
                    for sub in range(4):
                        r0 = grp * 512 + sub * 128
                        p, kp = psc.next()
                        for m in range(16):
                            I("pe", "matmul", p[:, m * 128:(m + 1) * 128], qT[:, m, sub * 128:(sub + 1) * 128], KT[:, m, :],
                              start=True, stop=True, r=[("qT", m), ("KT", 0), ("KT", 1)], w=[kp])
                        S, kS = S_r.next()
                        I("act", "copy", S[:, 0:1024], p[:, 0:1024], r=[kp], w=[(kS, 0)])
                        I("act", "copy", S[:, 1024:2048], p[:, 1024:2048], r=[kp], w=[(kS, 1)])
                        I("pool", "dma_start", out=Sd[r0:r0 + 128, :], in_=S[:], r=[(kS, 0), (kS, 1)], dma=True)
                        Sk = [(kS, 0), (kS, 1)]
                        S3 = S[:].rearrange("p (m i) -> p m i", m=16)
                        v, kv_ = v_r.next()
                        ix, kix = idx_r.next()
                        for m in range(16):
                            sw_, ksw = Swk_r.next()
                            I("dve", "max", out=v[:, m, 0:8], in_=S3[:, m, :], r=Sk, w=[kv_])
                            I("dve", "match_replace", out=sw_[:], in_to_replace=v[:, m, 0:8], in_values=S3[:, m, :],
                              imm_value=-1e30, r=Sk + [kv_], w=[ksw])
                            I("dve", "max", out=v[:, m, 8:16], in_=sw_[:], r=[ksw], w=[kv_])
                            if m % 2 == 0:
                                h = m // 2
                                I("dve", "max_index", out=ix[:, h, 0:8], in_max=v[:, m, 0:8], in_values=S3[:, m, :],
                                  r=Sk + [kv_], w=[kix])
                                I("dve", "max_index", out=ix[:, h, 8:16], in_max=v[:, m, 8:16], in_values=sw_[:],
                                  r=[ksw, kv_], w=[kix])
                        ixf, kixf = idxf_r.next()
                        I("dve", "tensor_copy", ixf[:], ix[:].rearrange("p h k -> p (h k)"), r=[kix], w=[kixf])
                        I("pool", "dma_start", out=Vd[r0:r0 + 128, :], in_=v[:].rearrange("p m k -> p (m k)"), r=[kv_], dma=True)
                        I("pool", "dma_start", out=Id[r0:r0 + 128, :], in_=ixf[:], r=[kixf], dma=True)
            P.barrier()

        if "B1b" in phases:
            with contextlib.ExitStack() as es:
                S2_r = Ring(es, nc, "S2b", 2, [128, 8, 128], F32)
                v_r = Ring(es, nc, "vb", 2, [128, 16, 16], F32)
                idxf_r = Ring(es, nc, "idxfb", 2, [128, 128], F32)
                idxT_r = Ring(es, nc, "idxT", 2, [128, 128], BF16)
                cand = sb(es, "cand", [128, 8, 256])
                cwk = sb(es, "cwk", [128, 8, 256])
                top = sb(es, "top", [128, 8, 16])
                tmp16 = sb(es, "tmp16", [128, 8, 16])
                zz_r = Ring(es, nc, "zz", 2, [128, 8], F32)
                c1_r = Ring(es, nc, "c1", 2, [128, 8, 16], F32)
                Xg_r = Ring(es, nc, "Xg", 2, [128, 128, 16], F32)
                Eg_r = Ring(es, nc, "Eg", 2, [128, 128, 16], BF16)
                Y = sb(es, "Yt", [128, 128, 128], BF16)
                YTs = sb(es, "YTs", [128, 128, 128], BF16)
                R_r = Ring(es, nc, "Rr", 2, [128, 128, 64], BF16)
                WT_r = Ring(es, nc, "WTs", 2, [128, 64, 128], BF16)
                pY = Ring(es, nc, "pY", 2, [128, 128, 4], F32, psum=True)
                pI = Ring(es, nc, "pI", 1, [128, 128], F32, psum=True)
                pW = Ring(es, nc, "pW", 4, [128, 64, 8], F32, psum=True)
                Ykeys = [("Y", h) for h in range(8)]
                YTkeys = [("YTs", ig) for ig in range(32)]

                def front(tt):
                    r0 = tt * 128
                    S2, kS = S2_r.next()
                    I("sp", "dma_start", out=S2[:],
                      in_=Sd[r0:r0 + 128, :].rearrange("p (h f i) -> p h f i", f=2, i=128)[:, :, 1, :], w=[kS], dma=True)
                    v, kv_ = v_r.next()
                    I("sp", "dma_start", out=v[:].rearrange("p m k -> p (m k)"), in_=Vd[r0:r0 + 128, :], w=[kv_], dma=True)
                    ixf, kixf = idxf_r.next()
                    I("sp", "dma_start", out=ixf[:], in_=Id[r0:r0 + 128, :], w=[kixf], dma=True)
                    v4 = v[:].rearrange("p (h f) k -> p h f k", f=2)
                    I("dve", "tensor_tensor", cand[:].rearrange("p h (a b) -> p h a b", a=16),
                      v4[:, :, 0, :].unsqueeze(3).to_broadcast([128, 8, 16, 16]),
                      v4[:, :, 1, :].unsqueeze(2).to_broadcast([128, 8, 16, 16]), ALU.add, r=[kv_], w=["cand"])
                    for h in range(8):
                        I("dve", "max", out=top[:, h, 0:8], in_=cand[:, h, :], r=["cand"], w=["top"])
                        I("dve", "match_replace", out=cwk[:, h, :], in_to_replace=top[:, h, 0:8], in_values=cand[:, h, :],
                          imm_value=-1e30, r=["cand", "top"], w=["cwk"])
                        I("dve", "max", out=top[:, h, 8:16], in_=cwk[:, h, :], r=["cwk"], w=["top"])
                    tau_b = top[:, :, 15:16].to_broadcast([128, 8, 16])
                    zz, kzz = zz_r.next()
                    c1, kc1 = c1_r.next()
                    I("dve", "tensor_tensor", tmp16[:], top[:], tau_b, ALU.subtract, r=["top"], w=["tmp16"])
                    I("dve", "tensor_tensor", c1[:], v4[:, :, 0, :], tau_b, ALU.subtract, r=[kv_, "top"], w=[kc1])
                    I("act", "activation", tmp16[:], tmp16[:], AF.Exp, r=["tmp16"], w=["tmp16"])
                    I("dve", "tensor_reduce", zz[:], tmp16[:], AX.X, ALU.add, r=["tmp16"], w=[kzz])
                    I("act", "activation", zz[:], zz[:], AF.Ln, r=[kzz], w=[kzz])
                    I("act", "mul", zz[:], zz[:], -1.0, r=[kzz], w=[kzz])
                    pi_, kpi = pI.next()
                    I("pe", "transpose", pi_[:], ixf[:], ident_f[:], r=[kixf, "ident_f"], w=[kpi])
                    idxT, kiT = idxT_r.next()
                    I("act", "copy", idxT[:], pi_[:], r=[kpi], w=[kiT])
                    return dict(S2=S2, kS=kS, zz=zz, kzz=kzz, c1=c1, kc1=kc1, idxT=idxT, kiT=kiT, tt=tt)

                def ybuild(f, heads=range(8)):
                    for h in heads:
                        Xg, kX = Xg_r.next()
                        I("pool", "tensor_tensor", Xg[:],
                          f["S2"][:, h, :].unsqueeze(2).to_broadcast([128, 128, 16]),
                          f["c1"][:, h, :].unsqueeze(1).to_broadcast([128, 128, 16]), ALU.add,
                          r=[f["kS"], f["kc1"]], w=[kX])
                        Eg, kE = Eg_r.next()
                        I("act", "activation", Eg[:], Xg[:], AF.Exp, bias=f["zz"][:, h:h + 1], r=[kX, f["kzz"]], w=[kE])
                        I("dve", "scalar_tensor_tensor", Y[:, :, h * 16:(h + 1) * 16], Xg[:], THR, Eg[:],
                          ALU.is_ge, ALU.mult, r=[kX, kE], w=[("Y", h)])

                def trans():
                    for ig in range(32):
                        p, kp = pY.next()
                        for ii in range(4):
                            i2 = ig * 4 + ii
                            I("pe", "matmul", p[:, :, ii], Y[:, i2, :], ident_b[:], start=True, stop=True,
                              r=Ykeys + ["ident_b"], w=[kp])
                        I("act", "copy", YTs[:, :, ig * 4:(ig + 1) * 4], p[:], r=[kp], w=[("YTs", ig)])

                def rbuild(f, ch):
                    R, kR = R_r.next()
                    I("dve", "tensor_tensor", R[:],
                      iota_cb[:, ch * 64:(ch + 1) * 64].unsqueeze(1).to_broadcast([128, 128, 64]),
                      f["idxT"][:].unsqueeze(2).to_broadcast([128, 128, 64]), ALU.is_equal,
                      r=["iota_cb", f["kiT"]], w=[kR])
                    return R, kR

                def mm(f, ch, R, kR, WT, kWT, tgs):
                    for tg in tgs:
                        p, kp = pW.next()
                        for ti_ in range(8):
                            t = tg * 8 + ti_
                            I("pe", "matmul", p[:, :, ti_], YTs[:, t, :], R[:, t, :], start=True, stop=True,
                              r=YTkeys + [kR], w=[kp])
                        I("act", "copy", WT[:, :, tg * 8:(tg + 1) * 8], p[:], r=[kp], w=[(kWT, tg)])

                def wstore(f, ch, WT, kWT):
                    I("sp", "dma_start", out=Wd[f["tt"]][:, ch * 8192:(ch + 1) * 8192],
                      in_=WT[:].rearrange("p c t -> p (c t)"), r=[(kWT, tg) for tg in range(16)], dma=True)

                f_cur = front(0)
                ybuild(f_cur)
                for tt in range(32):
                    trans()
                    f_next = front(tt + 1) if tt < 31 else None
                    Rs = [rbuild(f_cur, 0), None]
                    WTs = [WT_r.next(), WT_r.next()]
                    for h in range(8):
                        ch = h // 4
                        if h == 3:
                            Rs[1] = rbuild(f_cur, 1)
                        if f_next is not None:
                            ybuild(f_next, [h])
                        q = h % 4
                        mm(f_cur, ch, Rs[ch][0], Rs[ch][1], WTs[ch][0], WTs[ch][1], range(q * 4, q * 4 + 4))
                        if q == 3:
                            wstore(f_cur, ch, WTs[ch][0], WTs[ch][1])
                    f_cur = f_next
            P.barrier()

        if "B2" in phases:
            with contextlib.ExitStack() as es:
                gf = sb(es, "gf_s", [128, D])
                I("sp", "dma_start", out=gf[:], in_=gf_d[:, :], w=["gf"], dma=True)
                z2T_r = Ring(es, nc, "z2Tb", 2, [128, 16, 512], BF16)
                vst_r = Ring(es, nc, "vstb", 3, [128, D], F32)
                hacc = sb(es, "hacc", [128, 4, D])
                NB = 4
                ut_r = Ring(es, nc, "utb", 2, [128, NB, 2048], BF16)
                vb_r = Ring(es, nc, "vbb", 2, [128, NB, 2048], BF16)
                wb_r = Ring(es, nc, "wbb", 2, [128, 4, NB, 128], BF16)
                gl_r = Ring(es, nc, "gl", 2, [128, 512], BF16)
                pt_r = Ring(es, nc, "pt", 2, [128, NB, 512], BF16)
                pa = Ring(es, nc, "pa", 2, [128, 512], F32, psum=True)
                po = Ring(es, nc, "po2", 1, [128, 4, 512], F32, psum=True)
                ss_r = Ring(es, nc, "ss3", 4, [128, 2], F32)
                junk = sb(es, "junk", [128, D], BF16)
                ob_r = Ring(es, nc, "ob", 2, [128, D], F32)
                znx = z2T_r.next()
                I("sp", "dma_start", out=znx[0][:].rearrange("p a t -> p (a t)"), in_=Z2T[0], w=[znx[1]], dma=True)
                for grp in range(8):
                    z2T, kz2 = znx
                    if grp < 7:
                        znx = z2T_r.next()
                        I("sp", "dma_start", out=znx[0][:].rearrange("p a t -> p (a t)"), in_=Z2T[grp + 1], w=[znx[1]], dma=True)
                    nblk = 128 // NB
                    prev = None
                    for b in range(nblk + 1):
                        cur = None
                        if b < nblk:
                            c0 = b * NB
                            ut, kut = ut_r.next()
                            I("sp", "dma_start", out=ut[:], in_=UT[c0:c0 + NB].rearrange("c p f -> p c f"), w=[kut], dma=True)
                            vb, kvb = vb_r.next()
                            wb, kwb = wb_r.next()
                            I("sp", "dma_start", out=wb[:].rearrange("p s c t -> p s (c t)"),
                              in_=Wd[grp * 4:(grp + 1) * 4, :, c0 * 128:(c0 + NB) * 128].rearrange("s p f -> p s f"),
                              w=[kwb], dma=True)
                            pt, kpt = pt_r.next()
                            cur = (vb, kvb, pt, kpt)
                        for step in range(4):
                            if b < nblk:
                                c = step
                                p, kp = pa.next()
                                for dc in range(16):
                                    I("pe", "matmul", p[:], ut[:, c, dc * 128:(dc + 1) * 128], z2T[:, dc, :],
                                      start=(dc == 0), stop=(dc == 15), r=[kut, kz2], w=[kp])
                                gl, kgl = gl_r.next()
                                I("act", "activation", gl[:], p[:], AF.Gelu_apprx_tanh, r=[kp], w=[kgl])
                                vs_, kvs = vst_r.next()
                                I("sp", "dma_start", out=vs_[:], in_=peer_v[(c0 + c) * 128:(c0 + c + 1) * 128, :],
                                  w=[kvs], dma=True)
                                I("act", "copy", vb[:, c, :], vs_[:], r=[kvs], w=[(kvb, c)])
                                I("pool", "tensor_tensor", pt[:, c, :].rearrange("p (s t) -> p s t", s=4),
                                  gl[:].rearrange("p (s t) -> p s t", s=4), wb[:, :, c, :], ALU.mult,
                                  r=[kgl, kwb], w=[(kpt, c)])
                            if prev is not None:
                                pvb, pkvb, ppt, pkpt = prev
                                s = step
                                o, ko = po.next()
                                for c in range(NB):
                                    for cb in range(4):
                                        I("pe", "matmul", o[:, cb, :], ppt[:, c, s * 128:(s + 1) * 128],
                                          pvb[:, c, cb * 512:(cb + 1) * 512], start=(c == 0), stop=(c == NB - 1),
                                          r=[(pkpt, c), (pkvb, c)], w=[ko])
                                I("dve", "tensor_tensor", hacc[:, s, :], hacc[:, s, :], o[:].rearrange("p a b -> p (a b)"),
                                  ALU.add, r=[ko, ("hacc", s)], w=[("hacc", s)])
                        prev = cur
                        if b == 0:
                            I("sp", "dma_start", out=hacc[:],
                              in_=Hd[grp * 512:(grp + 1) * 512, :].rearrange("(s p) d -> p s d", p=128),
                              w=[("hacc", s) for s in range(4)], dma=True)
                    for s in range(4):
                        ss, kss = ss_r.next()
                        I("act", "activation", junk[:], hacc[:, s, :], AF.Square, accum_out=ss[:, 0:1],
                          r=[("hacc", s)], w=["junk", kss])
                        I("act", "activation", ss[:, 1:2], ss[:, 0:1], AF.Sqrt, scale=1.0 / D, bias=EPS, r=[kss], w=[kss])
                        I("dve", "reciprocal", ss[:, 1:2], ss[:, 1:2], r=[kss], w=[kss])
                        ob, kob = ob_r.next()
                        I("dve", "scalar_tensor_tensor", ob[:], hacc[:, s, :], ss[:, 1:2], gf[:], ALU.mult, ALU.mult,
                          r=[("hacc", s), kss, "gf"], w=[kob])
                        r0 = grp * 512 + s * 128
                        I("pool", "dma_start", out=out_d[r0:r0 + 128, :], in_=ob[:], r=[kob], dma=True)
        P.emit()
    return nc, P


def _chunkT(v):
    return np.ascontiguousarray(np.asarray(v, np.float32).reshape(-1, 128).T)


def make_in_maps(inp):
    x = np.asarray(inp["x"], np.float32)
    sw = np.zeros((128, 128), np.float32)
    sw[:, O_G1:O_G1 + 16] = _chunkT(inp["norm1_g"][0])
    sw[:, O_G2:O_G2 + 16] = _chunkT(inp["norm2_g"][0])
    sw[:, O_PB:O_PB + 8] = _chunkT(inp["pool_b"][0].reshape(-1))
    sw[:, O_PSC:O_PSC + 8] = _chunkT(inp["pool_scale"][0])
    for k in range(4):
        sw[:, O_CW + k * 8:O_CW + (k + 1) * 8] = _chunkT(inp["conv_w"][0][k])
    sw[:, O_CB:O_CB + 8] = _chunkT(inp["conv_b"][0])
    sw[:, O_GAB:O_GAB + 8] = _chunkT(inp["gate_a_b"][0].reshape(-1))
    sw[:, O_GXB:O_GXB + 8] = _chunkT(inp["gate_x_b"][0].reshape(-1))
    sw[:, O_LAM:O_LAM + 8] = _chunkT(inp["lru_lambda"][0])
    gf = np.ascontiguousarray(np.broadcast_to(np.asarray(inp["norm_f_g"], np.float32)[None, :], (128, D)))
    shared = {
        "gf": gf,
        "w_in": np.ascontiguousarray(inp["w_in"][0], dtype=np.float32),
        "pool_w": np.ascontiguousarray(inp["pool_w"][0], dtype=np.float32),
        "gate_a_w": np.ascontiguousarray(inp["gate_a_w"][0], dtype=np.float32),
        "gate_x_w": np.ascontiguousarray(inp["gate_x_w"][0], dtype=np.float32),
        "w_out": np.ascontiguousarray(inp["w_out"][0], dtype=np.float32),
        "peer_wq": np.ascontiguousarray(inp["peer_wq"][0], dtype=np.float32),
        "keys1": np.ascontiguousarray(inp["peer_keys1"][0], dtype=np.float32),
        "keys2": np.ascontiguousarray(inp["peer_keys2"][0], dtype=np.float32),
        "peer_u": np.ascontiguousarray(inp["peer_u"][0], dtype=np.float32),
        "peer_v": np.ascontiguousarray(inp["peer_v"][0], dtype=np.float32),
    }
    zeros = np.zeros((NTOK, D), np.float32)
    maps = []
    for core in range(8):
        b, half = core // 2, core % 2
        m = dict(shared)
        m["xo"] = np.ascontiguousarray(x[b, half * NTOK:(half + 1) * NTOK])
        m["xp"] = np.ascontiguousarray(x[b, 0:NTOK]) if half == 1 else zeros
        s = sw.copy()
        s[:, O_FLAG] = float(half)
        m["smallw"] = s
        pos = half * NTOK + np.arange(16, dtype=np.float32) + 1.0
        inv = np.stack([1.0 / np.minimum(pos, float(w)) for w in (2, 4, 8, 16)], 0).astype(np.float32)
        m["invdiv"] = np.ascontiguousarray(np.broadcast_to(inv[None], (128, 4, 16)))
        maps.append(m)
    return maps


def kernel(**inputs):
    nc, _ = build()
    maps = make_in_maps(inputs)
    res = run_bass_kernel_spmd(nc, maps, core_ids=list(range(8)))
    out = np.zeros((4, 8192, D), np.float32)
    for core in range(8):
        b, half = core // 2, core % 2
        out[b, half * NTOK:(half + 1) * NTOK] = np.asarray(res.results[core]["out"], np.float32)
    return out
```

```python
import contextlib
import numpy as np
import concourse.bass as bass
import concourse.mybir as mybir
from concourse.bass_utils import run_bass_kernel_spmd

F32 = mybir.dt.float32
BF16 = mybir.dt.bfloat16
U32 = mybir.dt.uint32
AF = mybir.ActivationFunctionType
ALU = mybir.AluOpType
AX = mybir.AxisListType

N_DMA_SEMS = 12
D = 2048
NTOK = 4096
NEXP = 16384
EPS = 1e-6
THR = -1e-5

DEBUG = False


class Prog:
    ENGS = ("pe", "act", "dve", "pool", "sp")

    def __init__(self, nc):
        self.nc = nc
        self.ops = []
        self.state = {}
        self.last_op = {}
        self.dmas_since_barrier = []
        self.pending = {}

    def _add(self, eng, fn, reads, writes, is_dma):
        idx = len(self.ops)
        deps = set()
        for k in reads:
            st = self.state.setdefault(k, {"w": {}, "r": {}})
            deps.update(st["w"].values())
        for k in writes:
            st = self.state.setdefault(k, {"w": {}, "r": {}})
            deps.update(st["w"].values())
            deps.update(st["r"].values())
        tag = (eng, idx) if is_dma else (eng,)
        for k in reads:
            self.state[k]["r"][tag] = idx
        for k in writes:
            st = self.state[k]
            st["w"] = {tag: idx}
            st["r"] = {}
        deps.discard(idx)
        deps |= self.pending.pop(eng, set())
        self.ops.append([eng, fn, deps, is_dma])
        if is_dma:
            self.dmas_since_barrier.append(idx)
        else:
            self.last_op[eng] = idx
        return idx

    def I(self, eng, meth, *args, r=(), w=(), dma=False, **kw):
        fn = lambda e: getattr(e, meth)(*args, **kw)
        return self._add(eng, fn, list(r), list(w), dma)

    def barrier(self):
        deps = set(self.last_op.values()) | set(self.dmas_since_barrier)
        for e in self.ENGS:
            self.pending[e] = set(deps) | self.pending.get(e, set())
        self.dmas_since_barrier = []
        self.state = {}

    def emit(self):
        nc = self.nc
        ops = self.ops
        needed = set()
        for i, (eng, fn, deps, is_dma) in enumerate(ops):
            nd = set()
            for d in deps:
                deng, _, _, d_dma = ops[d]
                if deng == eng and not d_dma and not is_dma and eng == "pe":
                    continue
                nd.add(d)
            ops[i][2] = nd
            needed.update(nd)
        with contextlib.ExitStack() as es:
            csem = {e: es.enter_context(nc.semaphore("c_" + e)) for e in self.ENGS}
            dsems = {e: [es.enter_context(nc.semaphore("d_%s_%d" % (e, j))) for j in range(N_DMA_SEMS)]
                     for e in ("sp", "pool")}
            ccount = {e: 0 for e in self.ENGS}
            dma_n = {e: 0 for e in dsems}
            dcount = {e: [0] * N_DMA_SEMS for e in dsems}
            sig = {}
            prev_dma = {}
            for i, (eng, fn, deps, is_dma) in enumerate(ops):
                if is_dma:
                    j = dma_n[eng] % N_DMA_SEMS
                    dma_n[eng] += 1
                    if dcount[eng][j] > 0:
                        prev_dma[i] = (dsems[eng][j], dcount[eng][j])
                    dcount[eng][j] += 16
                    sig[i] = (dsems[eng][j], dcount[eng][j])
                elif i in needed:
                    ccount[eng] += 1
                    sig[i] = (csem[eng], ccount[eng])
            per_eng = {e: [] for e in self.ENGS}
            for i, o in enumerate(ops):
                per_eng[o[0]].append(i)
            handles = {"pe": "tensor", "act": "scalar", "dve": "vector", "pool": "gpsimd", "sp": "sync"}
            self.n_waits = 0

            def make_section(ename):
                def section(e):
                    waited = {}
                    for i in per_eng[ename]:
                        eng, fn, deps, is_dma = ops[i]
                        wl = {}
                        for d in deps:
                            s, v = sig[d]
                            if wl.get(s.num, (None, 0))[1] < v:
                                wl[s.num] = (s, v)
                        if i in prev_dma:
                            s, v = prev_dma[i]
                            if wl.get(s.num, (None, 0))[1] < v:
                                wl[s.num] = (s, v)
                        for snum, (s, v) in wl.items():
                            if waited.get(snum, 0) < v:
                                e.wait_ge(s, v)
                                waited[snum] = v
                                self.n_waits += 1
                        ins = fn(e)
                        if i in sig:
                            ins.then_inc(sig[i][0], 16 if is_dma else 1)
                    if ename in dsems:
                        for j in range(N_DMA_SEMS):
                            if dcount[ename][j] > 0:
                                e.wait_ge(dsems[ename][j], dcount[ename][j])
                return section

            with nc.Block() as block:
                for ename in self.ENGS:
                    if per_eng[ename]:
                        getattr(block, handles[ename])(make_section(ename))
        return nc


class Ring:
    def __init__(self, es, nc, name, n, shape, dt, psum=False):
        alloc = nc.psum_tensor if psum else nc.sbuf_tensor
        self.t = [es.enter_context(alloc("%s%d" % (name, i), shape, dt)) for i in range(n)]
        self.name = name
        self.n = n
        self.i = -1

    def next(self):
        self.i += 1
        s = self.i % self.n
        return self.t[s], (self.name, s)

    def cur(self):
        s = self.i % self.n
        return self.t[s], (self.name, s)


O_G1, O_G2, O_PB, O_PSC, O_CW, O_CB, O_GAB, O_GXB, O_LAM, O_FLAG = 0, 16, 32, 40, 48, 80, 88, 96, 104, 112


def build(phases=("T", "A1", "A2", "B1a", "B1b", "B2"), dbg=()):
    nc = bass.Bass("TRN2", target_bir_lowering=False)

    def din(name, shape, dt=F32):
        return nc.dram_tensor(name, shape, dt, kind="ExternalInput").ap()

    xo = din("xo", [NTOK, D])
    xp = din("xp", [NTOK, D])
    smallw_d = din("smallw", [128, 128])
    gf_d = din("gf", [128, D])
    invdiv_d = din("invdiv", [128, 4, 16])
    w_in = din("w_in", [D, 3072])
    pool_w = din("pool_w", [4, 256, 256])
    gate_a_w = din("gate_a_w", [8, 128, 128])
    gate_x_w = din("gate_x_w", [8, 128, 128])
    w_out = din("w_out", [D, D])
    peer_wq = din("peer_wq", [D, D])
    keys1 = din("keys1", [8, 128, 128])
    keys2 = din("keys2", [8, 128, 128])
    peer_u = din("peer_u", [NEXP, D])
    peer_v = din("peer_v", [NEXP, D])
    out_d = nc.dram_tensor("out", [NTOK, D], F32, kind="ExternalOutput").ap()

    def scr(name, shape, dt):
        return nc.dram_tensor(name, shape, dt, kind=("ExternalOutput" if name in dbg else "Internal")).ap()

    UT = scr("UT", [128, 128, 2048], BF16)
    Vb = scr("Vb", [NEXP, D], BF16)
    YT = scr("YT", [8, 128, 16 * 512], BF16)
    Hd = scr("Hd", [NTOK, D], F32)
    Z2T = scr("Z2T", [8, 128, 16 * 512], BF16)
    Sd = scr("Sd", [NTOK, 2048], F32)
    Wd = scr("Wd", [32, 128, 128 * 128], BF16)
    Vd = scr("Vd", [NTOK, 256], F32)
    Id = scr("Id", [NTOK, 128], F32)

    P = Prog(nc)
    I = P.I

    with contextlib.ExitStack() as gs:
        def sb(es, name, shape, dt=F32):
            return es.enter_context(nc.sbuf_tensor(name, shape, dt))

        def ps(es, name, shape, dt=F32):
            return es.enter_context(nc.psum_tensor(name, shape, dt))

        ident_f = sb(gs, "ident_f", [128, 128])
        ident_b = sb(gs, "ident_b", [128, 128], BF16)
        iota_c = sb(gs, "iota_c", [128, 128])
        smallw = sb(gs, "smallw_s", [128, 128])
        cj = sb(gs, "cj", [128, 8])
        I("sp", "dma_start", out=smallw[:], in_=smallw_d[:, :], w=["smallw"], dma=True)
        I("pool", "iota", iota_c[:], [[1, 128]], base=0, channel_multiplier=-1,
          allow_small_or_imprecise_dtypes=True, w=["iota_c"])
        I("dve", "tensor_single_scalar", ident_f[:], iota_c[:], 0.0, ALU.is_equal, r=["iota_c"], w=["ident_f"])
        I("dve", "tensor_copy", ident_b[:], ident_f[:], r=["ident_f"], w=["ident_b"])
        I("pool", "iota", iota_c[:], [[1, 128]], base=0, channel_multiplier=0,
          allow_small_or_imprecise_dtypes=True, r=["iota_c"], w=["iota_c"])
        iota_cb = sb(gs, "iota_cb", [128, 128], BF16)
        I("dve", "tensor_copy", iota_cb[:], iota_c[:], r=["iota_c"], w=["iota_cb"])
        I("act", "activation", cj[:], smallw[:, O_LAM:O_LAM + 8], AF.Exp, scale=-1.0, r=["smallw"], w=["cj"])
        I("act", "activation", cj[:], cj[:], AF.Ln, bias=1.0, r=["cj"], w=["cj"])
        I("act", "mul", cj[:], cj[:], -8.0, r=["cj"], w=["cj"])
        hcon = sb(gs, "hcon", [128, 24])
        I("act", "mul", hcon[:, 0:16], smallw[:, O_GAB:O_GAB + 16], 0.5, r=["smallw"], w=["hcon"])
        I("act", "mul", hcon[:, 16:24], cj[:], 0.5, r=["cj", "hcon"], w=["hcon"])
        g1b = smallw[:, O_G1:O_G1 + 16].unsqueeze(2).to_broadcast([128, 16, 128])
        g2b = smallw[:, O_G2:O_G2 + 16].unsqueeze(2).to_broadcast([128, 16, 128])

        def rms_and_transpose(es_bufs, src_ap, src_key, dst, dst_key, col0, gb, tag, gcol=None):
            xn_r, ss_r, pTr_r = es_bufs
            xn, kxn = xn_r.next()
            ss, kss = ss_r.next()
            I("act", "activation", xn[:], src_ap, AF.Square, accum_out=ss[:, 0:1], r=[src_key], w=[kxn, kss])
            if gcol is None:
                I("act", "activation", ss[:, 1:2], ss[:, 0:1], AF.Sqrt, scale=1.0 / D, bias=EPS, r=[kss], w=[kss])
                I("dve", "reciprocal", ss[:, 1:2], ss[:, 1:2], r=[kss], w=[kss])
            else:
                I("act", "activation", ss[:, 1:2], ss[:, 0:1], AF.Ln, scale=1.0 / D, bias=EPS, r=[kss], w=[kss])
                I("act", "activation", ss[:, 1:2], ss[:, 1:2], AF.Exp, scale=-0.5, r=[kss], w=[kss])
            I("act", "activation", xn[:], src_ap, AF.Copy, scale=ss[:, 1:2], r=[src_key, kss], w=[kxn])
            pTr, kp = pTr_r.next()
            for dc in range(16):
                I("pe", "transpose", pTr[:, dc, :], xn[:, dc * 128:(dc + 1) * 128], ident_b[:],
                  r=[kxn, "ident_b"], w=[kp])
            if gcol is None:
                I("dve", "tensor_tensor", dst[:, :, col0:col0 + 128], pTr[:], gb, ALU.mult,
                  r=[kp, "smallw"], w=[dst_key])
            else:
                for dc in range(16):
                    I("act", "activation", dst[:, dc, col0:col0 + 128], pTr[:, dc, :], AF.Copy,
                      scale=smallw[:, gcol + dc:gcol + dc + 1], r=[kp, "smallw"], w=[dst_key])
            return ss

        if "T" in phases:
            with contextlib.ExitStack() as es:
                ust = Ring(es, nc, "ust", 2, [128, D], F32)
                vst = Ring(es, nc, "vst", 2, [128, D], F32)
                uts = Ring(es, nc, "uts", 2, [128, D], BF16)
                vbs = Ring(es, nc, "vbs", 2, [128, D], BF16)
                pT = Ring(es, nc, "pT", 2, [128, D], F32, psum=True)
                for c in range(128):
                    u, ku = ust.next()
                    I("sp", "dma_start", out=u[:], in_=peer_u[c * 128:(c + 1) * 128, :], w=[ku], dma=True)
                    p, kp = pT.next()
                    for dc in range(16):
                        I("pe", "transpose", p[:, dc * 128:(dc + 1) * 128], u[:, dc * 128:(dc + 1) * 128],
                          ident_f[:], r=[ku, "ident_f"], w=[kp])
                    o, ko = uts.next()
                    I("act", "copy", o[:, 0:1024], p[:, 0:1024], r=[kp], w=[(ko, 0)])
                    I("dve", "tensor_copy", o[:, 1024:2048], p[:, 1024:2048], r=[kp], w=[(ko, 1)])
                    I("pool", "dma_start", out=UT[c], in_=o[:], r=[(ko, 0), (ko, 1)], dma=True)
            P.barrier()

        if "A1" in phases:
            with contextlib.ExitStack() as es:
                W_in_b = sb(es, "W_in_b", [128, 16, 3072], BF16)
                xs = Ring(es, nc, "xs", 2, [128, D], F32)
                xn_r = Ring(es, nc, "xn", 1, [128, D], BF16)
                ss_r = Ring(es, nc, "ss", 4, [128, 2], F32)
                pTr_r = Ring(es, nc, "pTr", 1, [128, 16, 128], BF16, psum=True)
                zT_r = Ring(es, nc, "zT", 2, [128, 16, 512], BF16)
                pw_b = sb(es, "pw_b", [128, 8, 256], BF16)
                ga_b = sb(es, "ga_b", [128, 8, 128], BF16)
                gx_b = sb(es, "gx_b", [128, 8, 128], BF16)
                invdiv0 = sb(es, "invdiv0", [128, 4, 16])
                tmpd = sb(es, "tmpd", [128, 16])
                for dc in range(16):
                    for hf in range(2):
                        st, kst = xs.next()
                        I("sp", "dma_start", out=st[:, 0:1536],
                          in_=w_in[dc * 128:(dc + 1) * 128, hf * 1536:(hf + 1) * 1536], w=[kst], dma=True)
                        eng = "dve" if hf == 0 else "act"
                        if eng == "dve":
                            I("dve", "tensor_copy", W_in_b[:, dc, hf * 1536:(hf + 1) * 1536], st[:, 0:1536],
                              r=[kst], w=[("W_in", dc, hf)])
                        else:
                            I("act", "copy", W_in_b[:, dc, hf * 1536:(hf + 1) * 1536], st[:, 0:1536],
                              r=[kst], w=[("W_in", dc, hf)])
                st, kst = xs.next()
                I("sp", "dma_start", out=st[:, 0:2048].rearrange("p (a j) -> p a j", a=8),
                  in_=pool_w.rearrange("g (c p) j -> p (g c) j", p=128), w=[kst], dma=True)
                I("dve", "tensor_copy", pw_b[:], st[:, 0:2048].rearrange("p (a j) -> p a j", a=8), r=[kst], w=["pw_b"])
                st, kst = xs.next()
                I("sp", "dma_start", out=st[:, 0:1024].rearrange("p (a j) -> p a j", a=8),
                  in_=gate_a_w.rearrange("h i j -> i h j"), w=[kst], dma=True)
                I("dve", "tensor_copy", ga_b[:], st[:, 0:1024].rearrange("p (a j) -> p a j", a=8), r=[kst], w=["ga_b"])
                st, kst = xs.next()
                I("sp", "dma_start", out=st[:, 0:1024].rearrange("p (a j) -> p a j", a=8),
                  in_=gate_x_w.rearrange("h i j -> i h j"), w=[kst], dma=True)
                I("dve", "tensor_copy", gx_b[:], st[:, 0:1024].rearrange("p (a j) -> p a j", a=8), r=[kst], w=["gx_b"])
                I("sp", "dma_start", out=invdiv0[:], in_=invdiv_d[:, :, :], w=["invdiv0"], dma=True)
                W_in_keys = [("W_in", dc, hf) for dc in range(16) for hf in range(2)]

                pj = Ring(es, nc, "pj", 3, [128, 512], F32, psum=True)
                pg = Ring(es, nc, "pg", 2, [128, 512], F32, psum=True)
                pl = Ring(es, nc, "pl", 1, [128, 512], F32, psum=True)
                xl_r = Ring(es, nc, "xl", 2, [128, 515], F32)
                up_r = Ring(es, nc, "up", 4, [128, 528], F32)
                xhalo = sb(es, "xhalo", [128, 8, 3])
                uhalo = sb(es, "uhalo", [128, 8, 16])
                state = sb(es, "state", [128, 8])
                xc_r = Ring(es, nc, "xc", 2, [128, 512], F32)
                xcb_r = Ring(es, nc, "xcb", 1, [128, 512], BF16)
                r_r = Ring(es, nc, "rr", 2, [128, 512], F32)
                ig_r = Ring(es, nc, "ig", 2, [128, 512], F32)
                a_r = Ring(es, nc, "aa", 2, [128, 512], F32)
                hs_r = Ring(es, nc, "hs", 2, [128, 512], F32)
                gg_r = Ring(es, nc, "gg", 2, [128, 512], BF16)
                sA = sb(es, "sA", [128, 528])
                sB = sb(es, "sB", [128, 528])
                db_r = Ring(es, nc, "db", 4, [128, 512], BF16)
                yc_r = Ring(es, nc, "yc", 2, [128, 512], BF16)
                I("dve", "memset", xhalo[:], 0.0, w=[("xhalo", j) for j in range(8)])
                I("dve", "memset", uhalo[:], 0.0, w=[("uhalo", j) for j in range(8)])
                I("dve", "memset", state[:], 0.0, w=[("state", j) for j in range(8)])

                zcur = [None, None]

                def proj(cc):
                    p, kp = pj.next()
                    for dc in range(16):
                        I("pe", "matmul", p[:], W_in_b[:, dc, cc * 128:(cc + 1) * 128], zcur[0][:, dc, :],
                          start=(dc == 0), stop=(dc == 15), r=W_in_keys + [zcur[1]], w=[kp])
                    return p, kp

                def rms_tile(ti_):
                    src_ = xp if ti_ < 8 else xo
                    t0_ = (ti_ % 8) * 512
                    z, kz = zT_r.next()
                    for sub in range(4):
                        x_, kx = xs.next()
                        I("sp", "dma_start", out=x_[:], in_=src_[t0_ + sub * 128:t0_ + (sub + 1) * 128, :], w=[kx], dma=True)
                        rms_and_transpose((xn_r, ss_r, pTr_r), x_[:], kx, z, kz, sub * 128, g1b, "a1")
                    return z, kz

                def store_y(ti, ch, y, ky):
                    I("pool", "dma_start", out=YT[ti - 8][:, ch * 512:(ch + 1) * 512], in_=y[:], r=[ky], dma=True)

                znext = rms_tile(0)
                for ti in range(16):
                    prefix = ti < 8
                    zcur[0], zcur[1] = znext
                    def stage_x(j):
                        p, kp = proj(8 + j)
                        xl, kxl = xl_r.next()
                        I("dve", "tensor_copy", xl[:, 0:3], xhalo[:, j, :], r=[("xhalo", j)], w=[(kxl, 0)])
                        I("act", "copy", xl[:, 3:515], p[:], r=[kp], w=[(kxl, 1)])
                        return xl, [(kxl, 0), (kxl, 1)]

                    def stageA(j, xl, kxl):
                        d = {}
                        if not prefix:
                            p2, kp2 = proj(16 + j)
                            gg, kgg = gg_r.next()
                            I("act", "activation", gg[:], p2[:], AF.Gelu_apprx_tanh, r=[kp2], w=[kgg])
                            d["gg"], d["kgg"] = gg, kgg
                        xc, kxc = xc_r.next()
                        cw = lambda k: smallw[:, O_CW + k * 8 + j:O_CW + k * 8 + j + 1]
                        I("dve", "tensor_scalar", xc[:], xl[:, 0:512], cw(0), smallw[:, O_CB + j:O_CB + j + 1],
                          ALU.mult, ALU.add, r=kxl + ["smallw"], w=[kxc])
                        for k in range(1, 4):
                            I("dve", "scalar_tensor_tensor", xc[:], xl[:, k:k + 512], cw(k), xc[:], ALU.mult, ALU.add,
                              r=kxl + [kxc, "smallw"], w=[kxc])
                        I("dve", "tensor_copy", xhalo[:, j, :], xl[:, 512:515], r=kxl, w=[("xhalo", j)])
                        xcb, kxcb = xcb_r.next()
                        I("act", "copy", xcb[:], xc[:], r=[kxc], w=[kxcb])
                        pr, kpr = pg.next()
                        I("pe", "matmul", pr[:], ga_b[:, j, :], xcb[:], start=True, stop=True, r=["ga_b", kxcb], w=[kpr])
                        pi, kpi = pg.next()
                        I("pe", "matmul", pi[:], gx_b[:, j, :], xcb[:], start=True, stop=True, r=["gx_b", kxcb], w=[kpi])
                        rr, krr = r_r.next()
                        I("act", "activation", rr[:], pr[:], AF.Tanh, scale=0.5, bias=hcon[:, j:j + 1],
                          r=[kpr, "hcon"], w=[krr])
                        ig, kig = ig_r.next()
                        I("act", "activation", ig[:], pi[:], AF.Tanh, scale=0.5, bias=hcon[:, 8 + j:9 + j],
                          r=[kpi, "hcon"], w=[kig])
                        aa, kaa = a_r.next()
                        I("act", "activation", aa[:], rr[:], AF.Exp, scale=hcon[:, 16 + j:17 + j], bias=hcon[:, 16 + j:17 + j],
                          r=[krr, "hcon"], w=[kaa])
                        mm, kmm = rr, krr
                        I("act", "activation", mm[:], rr[:], AF.Exp, scale=cj[:, j:j + 1], bias=cj[:, j:j + 1],
                          r=[krr, "cj"], w=[kmm])
                        I("act", "activation", mm[:], mm[:], AF.Sqrt, scale=-1.0, bias=1.0, r=[kmm], w=[kmm])
                        d.update(xc=xc, kxc=kxc, ig=ig, kig=kig, aa=aa, kaa=kaa, mm=mm, kmm=kmm, j=j)
                        return d

                    def stageB(d):
                        j = d["j"]
                        ig, kig = d["ig"], d["kig"]
                        I("dve", "scalar_tensor_tensor", ig[:], ig[:], 1.0, d["xc"][:], ALU.add, ALU.mult,
                          r=[kig, d["kxc"]], w=[kig])
                        I("dve", "scalar_tensor_tensor", ig[:], ig[:], 0.5, d["mm"][:], ALU.mult, ALU.mult,
                          r=[kig, d["kmm"]], w=[kig])
                        hs, khs = hs_r.next()
                        I("dve", "tensor_tensor_scan", hs[:], d["aa"][:], ig[:], state[:, j:j + 1], ALU.mult, ALU.add,
                          r=[d["kaa"], kig, ("state", j)], w=[khs])
                        if ti == 7:
                            I("dve", "tensor_tensor", state[:, j:j + 1], hs[:, 511:512],
                              smallw[:, O_FLAG:O_FLAG + 1], ALU.mult, r=[khs, "smallw"], w=[("state", j)])
                        else:
                            I("dve", "tensor_copy", state[:, j:j + 1], hs[:, 511:512], r=[khs], w=[("state", j)])
                        if not prefix:
                            y, ky = yc_r.next()
                            I("dve", "tensor_tensor", y[:], hs[:], d["gg"][:], ALU.mult, r=[khs, d["kgg"]], w=[ky])
                            store_y(ti, 8 + j, y, ky)

                    nxt = stage_x(0)
                    dprev = None
                    for j in range(9):
                        dcur = None
                        if j < 8:
                            xl, kxl = nxt
                            nxt = stage_x(j + 1) if j < 7 else None
                            dcur = stageA(j, xl, kxl)
                        if dprev is not None:
                            stageB(dprev)
                        dprev = dcur
                    zkeep = (zcur[0], zcur[1])
                    if ti < 15:
                        znext = rms_tile(ti + 1)
                    zcur[0], zcur[1] = zkeep
                    if ti >= 7:
                        def stage_u(ch):
                            p, kp = proj(ch)
                            up, kup = up_r.next()
                            I("dve", "tensor_copy", up[:, 0:16], uhalo[:, ch, :], r=[("uhalo", ch)], w=[(kup, 0)])
                            I("act", "copy", up[:, 16:528], p[:], r=[kp], w=[(kup, 1)])
                            return up, [(kup, 0), (kup, 1)]

                        nxt = [stage_u(0), stage_u(1)]
                        for g in range(4):
                            ups = nxt
                            nxt = [stage_u(2 * g + 2), stage_u(2 * g + 3)] if g < 3 else None
                            for ic in range(2):
                                I("dve", "tensor_copy", uhalo[:, 2 * g + ic, :], ups[ic][0][:, 512:528], r=ups[ic][1],
                                  w=[("uhalo", 2 * g + ic)])
                            if prefix:
                                continue
                            dbs = []
                            for ic in range(2):
                                up, kup = ups[ic]
                                cur, kcur = up, kup
                                bufs = [(sA, "sA"), (sB, "sB")]
                                sh = 1
                                for lvl in range(g + 1):
                                    nb, knb = bufs[lvl % 2]
                                    lo = 2 * sh - 1
                                    I("pool", "tensor_tensor", nb[:, lo:528], cur[:, lo:528], cur[:, lo - sh:528 - sh], ALU.add,
                                      r=kcur, w=[knb])
                                    cur, kcur = nb, [knb]
                                    sh *= 2
                                w = float(2 ** (g + 1))
                                d_, kd = db_r.next()
                                I("dve", "scalar_tensor_tensor", d_[:], cur[:, 16:528], 1.0 / w, up[:, 16:528],
                                  ALU.mult, ALU.subtract, r=kcur + kup, w=[kd])
                                if ti == 8:
                                    I("dve", "tensor_tensor", tmpd[:], cur[:, 16:32], invdiv0[:, g, :], ALU.mult,
                                      r=kcur + ["invdiv0"], w=["tmpd"])
                                    I("dve", "tensor_tensor", d_[:, 0:16], tmpd[:], up[:, 16:32], ALU.subtract,
                                      r=["tmpd"] + kup, w=[kd])
                                dbs.append((d_, kd))
                            for jo in range(2):
                                ch = g * 2 + jo
                                pp, kpp = pl.next()
                                for ic in range(2):
                                    I("pe", "matmul", pp[:], pw_b[:, g * 2 + ic, jo * 128:(jo + 1) * 128], dbs[ic][0][:],
                                      start=(ic == 0), stop=(ic == 1), r=["pw_b", dbs[ic][1]], w=[kpp])
                                y, ky = yc_r.next()
                                I("dve", "tensor_scalar", y[:], pp[:], smallw[:, O_PB + ch:O_PB + ch + 1],
                                  smallw[:, O_PSC + ch:O_PSC + ch + 1], ALU.add, ALU.mult, r=[kpp, "smallw"], w=[ky])
                                store_y(ti, ch, y, ky)
            P.barrier()

        if "A2" in phases:
            with contextlib.ExitStack() as es:
                W_out_b = sb(es, "W_out_b", [128, 16, D], BF16)
                xs = Ring(es, nc, "xs2", 2, [128, D], F32)
                yT_r = Ring(es, nc, "yTr", 2, [128, 16, 512], BF16)
                hs_r = Ring(es, nc, "hsub", 2, [128, D], F32)
                po = Ring(es, nc, "po", 4, [128, 512], F32, psum=True)
                for mc in range(16):
                    st, kst = xs.next()
                    I("sp", "dma_start", out=st[:], in_=w_out[mc * 128:(mc + 1) * 128, :], w=[kst], dma=True)
                    if mc % 2 == 0:
                        I("dve", "tensor_copy", W_out_b[:, mc, :], st[:], r=[kst], w=[("W_out", mc)])
                    else:
                        I("act", "copy", W_out_b[:, mc, :], st[:], r=[kst], w=[("W_out", mc)])
                W_out_keys = [("W_out", mc) for mc in range(16)]
                for ti in range(8):
                    yT, kyT = yT_r.next()
                    I("sp", "dma_start", out=yT[:].rearrange("p a t -> p (a t)"), in_=YT[ti], w=[kyT], dma=True)
                    for sub in range(4):
                        r0 = ti * 512 + sub * 128
                        x_, kx = xs.next()
                        I("sp", "dma_start", out=x_[:], in_=xo[r0:r0 + 128, :], w=[kx], dma=True)
                        h_, kh = hs_r.next()
                        for cb in range(4):
                            p, kp = po.next()
                            for mc in range(16):
                                I("pe", "matmul", p[:], yT[:, mc, sub * 128:(sub + 1) * 128],
                                  W_out_b[:, mc, cb * 512:(cb + 1) * 512], start=(mc == 0), stop=(mc == 15),
                                  r=[kyT] + W_out_keys, w=[kp])
                            I("dve", "tensor_tensor", h_[:, cb * 512:(cb + 1) * 512], p[:], x_[:, cb * 512:(cb + 1) * 512],
                              ALU.add, r=[kp, kx], w=[(kh, cb)])
                        I("pool", "dma_start", out=Hd[r0:r0 + 128, :], in_=h_[:], r=[(kh, cb) for cb in range(4)], dma=True)
            P.barrier()

        if "B1a" in phases:
            with contextlib.ExitStack() as es:
                Wq_b = sb(es, "Wq_b", [128, 16, D], BF16)
                KT = sb(es, "KT", [128, 16, 128], BF16)
                hin = Ring(es, nc, "hin", 2, [128, D], F32)
                xn_r = Ring(es, nc, "xn2", 2, [128, D], BF16)
                ss_r = Ring(es, nc, "ss2", 4, [128, 2], F32)
                pTr_r = Ring(es, nc, "pTr2", 1, [128, 16, 128], BF16, psum=True)
                z2T_r = Ring(es, nc, "z2T", 2, [128, 16, 512], BF16)
                qT = sb(es, "qT", [128, 16, 512], BF16)
                pq = Ring(es, nc, "pq", 2, [128, 512], F32, psum=True)
                psc = Ring(es, nc, "psc", 1, [128, 2048], F32, psum=True)
                S_r = Ring(es, nc, "Ss", 5, [128, 2048], F32)
                Swk_r = Ring(es, nc, "Swk", 2, [128, 128], F32)
                v_r = Ring(es, nc, "v1", 5, [128, 16, 16], F32)
                idx_r = Ring(es, nc, "idx1", 5, [128, 8, 16], U32)
                idxf_r = Ring(es, nc, "idxf1", 5, [128, 128], F32)
                for mc in range(16):
                    st, kst = hin.next()
                    I("sp", "dma_start", out=st[:], in_=peer_wq[mc * 128:(mc + 1) * 128, :], w=[kst], dma=True)
                    if mc % 2 == 0:
                        I("dve", "tensor_copy", Wq_b[:, mc, :], st[:], r=[kst], w=[("Wq", mc)])
                    else:
                        I("act", "copy", Wq_b[:, mc, :], st[:], r=[kst], w=[("Wq", mc)])
                Wq_keys = [("Wq", mc) for mc in range(16)]
                for half, kd in ((0, keys1), (1, keys2)):
                    st, kst = hin.next()
                    I("sp", "dma_start", out=st[:, 0:1024].rearrange("p (h d) -> p h d", h=8),
                      in_=kd.rearrange("h i d -> i h d"), w=[kst], dma=True)
                    p, kp = psc.next()
                    for h in range(8):
                        I("pe", "transpose", p[:, h * 128:(h + 1) * 128], st[:, h * 128:(h + 1) * 128], ident_f[:],
                          r=[kst, "ident_f"], w=[kp])
                    I("dve", "tensor_copy", KT[:].rearrange("p (h f) i -> p h f i", f=2)[:, :, half, :],
                      p[:, 0:1024].rearrange("p (h i) -> p h i", h=8), r=[kp], w=[("KT", half)])
                for grp in range(8):
                    z2T, kz = z2T_r.next()
                    for sub in range(4):
                        r0 = grp * 512 + sub * 128
                        h_, kh = hin.next()
                        I("sp", "dma_start", out=h_[:], in_=Hd[r0:r0 + 128, :], w=[kh], dma=True)
                        rms_and_transpose((xn_r, ss_r, pTr_r), h_[:], kh, z2T, kz, sub * 128, g2b, "b1", gcol=O_G2)
                    I("pool", "dma_start", out=Z2T[grp], in_=z2T[:].rearrange("p a t -> p (a t)"), r=[kz], dma=True)
                    for m in range(16):
                        p, kp = pq.next()
                        for dc in range(16):
                            I("pe", "matmul", p[:], Wq_b[:, dc, m * 128:(m + 1) * 128], z2T[:, dc, :],
                              start=(dc == 0), stop=(dc == 15), r=Wq_keys + [kz], w=[kp])
                        I("act", "copy", qT[:, m, :], p[:], r=[kp], w=[("qT", m)])
                    for sub in range(4):
                        r0 = grp * 512 + sub * 128
                        p, kp = psc.next()
                        for m in range(16):
                            I("pe", "matmul", p[:, m * 128:(m + 1) * 128], qT[:, m, sub * 128:(sub + 1) * 128], KT[:, m, :],
                              start=True, stop=True, r=[("qT", m), ("KT", 0), ("KT", 1)], w=[kp])
                        S, kS = S_r.next()
                        I("act", "copy", S[:, 0:1024], p[:, 0:1024], r=[kp], w=[(kS, 0)])
                        I("act", "copy", S[:, 1024:2048], p[:, 1024:2048], r=[kp], w=[(kS, 1)])
                        I("pool", "dma_start", out=Sd[r0:r0 + 128, :], in_=S[:], r=[(kS, 0), (kS, 1)], dma=True)
                        Sk = [(kS, 0), (kS, 1)]
                        S3 = S[:].rearrange("p (m i) -> p m i", m=16)
                        v, kv_ = v_r.next()
                        ix, kix = idx_r.next()
                        for m in range(16):
                            sw_, ksw = Swk_r.next()
                            I("dve", "max", out=v[:, m, 0:8], in_=S3[:, m, :], r=Sk, w=[kv_])
                            I("dve", "match_replace", out=sw_[:], in_to_replace=v[:, m, 0:8], in_values=S3[:, m, :],
                              imm_value=-1e30, r=Sk + [kv_], w=[ksw])
                            I("dve", "max", out=v[:, m, 8:16], in_=sw_[:], r=[ksw], w=[kv_])
                            if m % 2 == 0:
                                h = m // 2
                                I("dve", "max_index", out=ix[:, h, 0:8], in_max=v[:, m, 0:8], in_values=S3[:, m, :],
                                  r=Sk + [kv_], w=[kix])
                                I("dve", "max_index", out=ix[:, h, 8:16], in_max=v[:, m, 8:16], in_values=sw_[:],
                                  r=[ksw, kv_], w=[kix])
                        ixf, kixf = idxf_r.next()
                        I("dve", "tensor_copy", ixf[:], ix[:].rearrange("p h k -> p (h k)"), r=[kix], w=[kixf])
                        I("pool", "dma_start", out=Vd[r0:r0 + 128, :], in_=v[:].rearrange("p m k -> p (m k)"), r=[kv_], dma=True)
                        I("pool", "dma_start", out=Id[r0:r0 + 128, :], in_=ixf[:], r=[kixf], dma=True)
            P.barrier()

        if "B1b" in phases:
            with contextlib.ExitStack() as es:
                S2_r = Ring(es, nc, "S2b", 2, [128, 8, 128], F32)
                v_r = Ring(es, nc, "vb", 2, [128, 16, 16], F32)
                idxf_r = Ring(es, nc, "idxfb", 2, [128, 128], F32)
                idxT_r = Ring(es, nc, "idxT", 2, [128, 128], BF16)
                cand = sb(es, "cand", [128, 8, 256])
                cwk = sb(es, "cwk", [128, 8, 256])
                top = sb(es, "top", [128, 8, 16])
                tmp16 = sb(es, "tmp16", [128, 8, 16])
                zz_r = Ring(es, nc, "zz", 2, [128, 8], F32)
                c1_r = Ring(es, nc, "c1", 2, [128, 8, 16], F32)
                Xg_r = Ring(es, nc, "Xg", 2, [128, 128, 16], F32)
                Eg_r = Ring(es, nc, "Eg", 2, [128, 128, 16], BF16)
                Y = sb(es, "Yt", [128, 128, 128], BF16)
                YTs = sb(es, "YTs", [128, 128, 128], BF16)
                R_r = Ring(es, nc, "Rr", 2, [128, 128, 64], BF16)
                WT_r = Ring(es, nc, "WTs", 2, [128, 64, 128], BF16)
                pY = Ring(es, nc, "pY", 2, [128, 128, 4], F32, psum=True)
                pI = Ring(es, nc, "pI", 1, [128, 128], F32, psum=True)
                pW = Ring(es, nc, "pW", 4, [128, 64, 8], F32, psum=True)
                Ykeys = [("Y", h) for h in range(8)]
                YTkeys = [("YTs", ig) for ig in range(32)]

                def front(tt):
                    r0 = tt * 128
                    S2, kS = S2_r.next()
                    I("sp", "dma_start", out=S2[:],
                      in_=Sd[r0:r0 + 128, :].rearrange("p (h f i) -> p h f i", f=2, i=128)[:, :, 1, :], w=[kS], dma=True)
                    v, kv_ = v_r.next()
                    I("sp", "dma_start", out=v[:].rearrange("p m k -> p (m k)"), in_=Vd[r0:r0 + 128, :], w=[kv_], dma=True)
                    ixf, kixf = idxf_r.next()
                    I("sp", "dma_start", out=ixf[:], in_=Id[r0:r0 + 128, :], w=[kixf], dma=True)
                    v4 = v[:].rearrange("p (h f) k -> p h f k", f=2)
                    I("dve", "tensor_tensor", cand[:].rearrange("p h (a b) -> p h a b", a=16),
                      v4[:, :, 0, :].unsqueeze(3).to_broadcast([128, 8, 16, 16]),
                      v4[:, :, 1, :].unsqueeze(2).to_broadcast([128, 8, 16, 16]), ALU.add, r=[kv_], w=["cand"])
                    for h in range(8):
                        I("dve", "max", out=top[:, h, 0:8], in_=cand[:, h, :], r=["cand"], w=["top"])
                        I("dve", "match_replace", out=cwk[:, h, :], in_to_replace=top[:, h, 0:8], in_values=cand[:, h, :],
                          imm_value=-1e30, r=["cand", "top"], w=["cwk"])
                        I("dve", "max", out=top[:, h, 8:16], in_=cwk[:, h, :], r=["cwk"], w=["top"])
                    tau_b = top[:, :, 15:16].to_broadcast([128, 8, 16])
                    zz, kzz = zz_r.next()
                    c1, kc1 = c1_r.next()
                    I("dve", "tensor_tensor", tmp16[:], top[:], tau_b, ALU.subtract, r=["top"], w=["tmp16"])
                    I("dve", "tensor_tensor", c1[:], v4[:, :, 0, :], tau_b, ALU.subtract, r=[kv_, "top"], w=[kc1])
                    I("act", "activation", tmp16[:], tmp16[:], AF.Exp, r=["tmp16"], w=["tmp16"])
                    I("dve", "tensor_reduce", zz[:], tmp16[:], AX.X, ALU.add, r=["tmp16"], w=[kzz])
                    I("act", "activation", zz[:], zz[:], AF.Ln, r=[kzz], w=[kzz])
                    I("act", "mul", zz[:], zz[:], -1.0, r=[kzz], w=[kzz])
                    pi_, kpi = pI.next()
                    I("pe", "transpose", pi_[:], ixf[:], ident_f[:], r=[kixf, "ident_f"], w=[kpi])
                    idxT, kiT = idxT_r.next()
                    I("act", "copy", idxT[:], pi_[:], r=[kpi], w=[kiT])
                    return dict(S2=S2, kS=kS, zz=zz, kzz=kzz, c1=c1, kc1=kc1, idxT=idxT, kiT=kiT, tt=tt)

                def ybuild(f, heads=range(8)):
                    for h in heads:
                        Xg, kX = Xg_r.next()
                        I("pool", "tensor_tensor", Xg[:],
                          f["S2"][:, h, :].unsqueeze(2).to_broadcast([128, 128, 16]),
                          f["c1"][:, h, :].unsqueeze(1).to_broadcast([128, 128, 16]), ALU.add,
                          r=[f["kS"], f["kc1"]], w=[kX])
                        Eg, kE = Eg_r.next()
                        I("act", "activation", Eg[:], Xg[:], AF.Exp, bias=f["zz"][:, h:h + 1], r=[kX, f["kzz"]], w=[kE])
                        I("dve", "scalar_tensor_tensor", Y[:, :, h * 16:(h + 1) * 16], Xg[:], THR, Eg[:],
                          ALU.is_ge, ALU.mult, r=[kX, kE], w=[("Y", h)])

                def trans():
                    for ig in range(32):
                        p, kp = pY.next()
                        for ii in range(4):
                            i2 = ig * 4 + ii
                            I("pe", "matmul", p[:, :, ii], Y[:, i2, :], ident_b[:], start=True, stop=True,
                              r=Ykeys + ["ident_b"], w=[kp])
                        I("act", "copy", YTs[:, :, ig * 4:(ig + 1) * 4], p[:], r=[kp], w=[("YTs", ig)])

                def rbuild(f, ch):
                    R, kR = R_r.next()
                    I("dve", "tensor_tensor", R[:],
                      iota_cb[:, ch * 64:(ch + 1) * 64].unsqueeze(1).to_broadcast([128, 128, 64]),
                      f["idxT"][:].unsqueeze(2).to_broadcast([128, 128, 64]), ALU.is_equal,
                      r=["iota_cb", f["kiT"]], w=[kR])
                    return R, kR

                def mm(f, ch, R, kR, WT, kWT, tgs):
                    for tg in tgs:
                        p, kp = pW.next()
                        for ti_ in range(8):
                            t = tg * 8 + ti_
                            I("pe", "matmul", p[:, :, ti_], YTs[:, t, :], R[:, t, :], start=True, stop=True,
                              r=YTkeys + [kR], w=[kp])
                        I("act", "copy", WT[:, :, tg * 8:(tg + 1) * 8], p[:], r=[kp], w=[(kWT, tg)])

                def wstore(f, ch, WT, kWT):
                    I("sp", "dma_start", out=Wd[f["tt"]][:, ch * 8192:(ch + 1) * 8192],
                      in_=WT[:].rearrange("p c t -> p (c t)"), r=[(kWT, tg) for tg in range(16)], dma=True)

                f_cur = front(0)
                ybuild(f_cur)
                for tt in range(32):
                    trans()
                    f_next = front(tt + 1) if tt < 31 else None
                    Rs = [rbuild(f_cur, 0), None]
                    WTs = [WT_r.next(), WT_r.next()]
                    for h in range(8):
                        ch = h // 4
                        if h == 3:
                            Rs[1] = rbuild(f_cur, 1)
                        if f_next is not None:
                            ybuild(f_next, [h])
                        q = h % 4
                        mm(f_cur, ch, Rs[ch][0], Rs[ch][1], WTs[ch][0], WTs[ch][1], range(q * 4, q * 4 + 4))
                        if q == 3:
                            wstore(f_cur, ch, WTs[ch][0], WTs[ch][1])
                    f_cur = f_next
            P.barrier()

        if "B2" in phases:
            with contextlib.ExitStack() as es:
                gf = sb(es, "gf_s", [128, D])
                I("sp", "dma_start", out=gf[:], in_=gf_d[:, :], w=["gf"], dma=True)
                z2T_r = Ring(es, nc, "z2Tb", 2, [128, 16, 512], BF16)
                vst_r = Ring(es, nc, "vstb", 3, [128, D], F32)
                hacc = sb(es, "hacc", [128, 4, D])
                NB = 4
                ut_r = Ring(es, nc, "utb", 2, [128, NB, 2048], BF16)
                vb_r = Ring(es, nc, "vbb", 2, [128, NB, 2048], BF16)
                wb_r = Ring(es, nc, "wbb", 2, [128, 4, NB, 128], BF16)
                gl_r = Ring(es, nc, "gl", 2, [128, 512], BF16)
                pt_r = Ring(es, nc, "pt", 2, [128, NB, 512], BF16)
                pa = Ring(es, nc, "pa", 2, [128, 512], F32, psum=True)
                po = Ring(es, nc, "po2", 1, [128, 4, 512], F32, psum=True)
                ss_r = Ring(es, nc, "ss3", 4, [128, 2], F32)
                junk = sb(es, "junk", [128, D], BF16)
                ob_r = Ring(es, nc, "ob", 2, [128, D], F32)
                znx = z2T_r.next()
                I("sp", "dma_start", out=znx[0][:].rearrange("p a t -> p (a t)"), in_=Z2T[0], w=[znx[1]], dma=True)
                for grp in range(8):
                    z2T, kz2 = znx
                    if grp < 7:
                        znx = z2T_r.next()
                        I("sp", "dma_start", out=znx[0][:].rearrange("p a t -> p (a t)"), in_=Z2T[grp + 1], w=[znx[1]], dma=True)
                    nblk = 128 // NB
                    prev = None
                    for b in range(nblk + 1):
                        cur = None
                        if b < nblk:
                            c0 = b * NB
                            ut, kut = ut_r.next()
                            I("sp", "dma_start", out=ut[:], in_=UT[c0:c0 + NB].rearrange("c p f -> p c f"), w=[kut], dma=True)
                            vb, kvb = vb_r.next()
                            wb, kwb = wb_r.next()
                            I("sp", "dma_start", out=wb[:].rearrange("p s c t -> p s (c t)"),
                              in_=Wd[grp * 4:(grp + 1) * 4, :, c0 * 128:(c0 + NB) * 128].rearrange("s p f -> p s f"),
                              w=[kwb], dma=True)
                            pt, kpt = pt_r.next()
                            cur = (vb, kvb, pt, kpt)
                        if b == 1:
                            I("sp", "dma_start", out=hacc[:],
                              in_=Hd[grp * 512:(grp + 1) * 512, :].rearrange("(s p) d -> p s d", p=128),
                              w=[("hacc", s) for s in range(4)], dma=True)
                        for step in range(4):
                            if b < nblk:
                                c = step
                                p, kp = pa.next()
                                for dc in range(16):
                                    I("pe", "matmul", p[:], ut[:, c, dc * 128:(dc + 1) * 128], z2T[:, dc, :],
                                      start=(dc == 0), stop=(dc == 15), r=[kut, kz2], w=[kp])
                                gl, kgl = gl_r.next()
                                I("act", "activation", gl[:], p[:], AF.Gelu_apprx_tanh, r=[kp], w=[kgl])
                                vs_, kvs = vst_r.next()
                                I("sp", "dma_start", out=vs_[:], in_=peer_v[(c0 + c) * 128:(c0 + c + 1) * 128, :],
                                  w=[kvs], dma=True)
                                I("act", "copy", vb[:, c, :], vs_[:], r=[kvs], w=[(kvb, c)])
                                I("pool", "tensor_tensor", pt[:, c, :].rearrange("p (s t) -> p s t", s=4),
                                  gl[:].rearrange("p (s t) -> p s t", s=4), wb[:, :, c, :], ALU.mult,
                                  r=[kgl, kwb], w=[(kpt, c)])
                            if prev is not None:
                                pvb, pkvb, ppt, pkpt = prev
                                s = step
                                o, ko = po.next()
                                for c in range(NB):
                                    for cb in range(4):
                                        I("pe", "matmul", o[:, cb, :], ppt[:, c, s * 128:(s + 1) * 128],
                                          pvb[:, c, cb * 512:(cb + 1) * 512], start=(c == 0), stop=(c == NB - 1),
                                          r=[(pkpt, c), (pkvb, c)], w=[ko])
                                I("dve", "tensor_tensor", hacc[:, s, :], hacc[:, s, :], o[:].rearrange("p a b -> p (a b)"),
                                  ALU.add, r=[ko, ("hacc", s)], w=[("hacc", s)])
                        prev = cur
                    for s in range(4):
                        ss, kss = ss_r.next()
                        I("act", "activation", junk[:], hacc[:, s, :], AF.Square, accum_out=ss[:, 0:1],
                          r=[("hacc", s)], w=["junk", kss])
                        I("act", "activation", ss[:, 1:2], ss[:, 0:1], AF.Sqrt, scale=1.0 / D, bias=EPS, r=[kss], w=[kss])
                        I("dve", "reciprocal", ss[:, 1:2], ss[:, 1:2], r=[kss], w=[kss])
                        ob, kob = ob_r.next()
                        I("dve", "scalar_tensor_tensor", ob[:], hacc[:, s, :], ss[:, 1:2], gf[:], ALU.mult, ALU.mult,
                          r=[("hacc", s), kss, "gf"], w=[kob])
                        r0 = grp * 512 + s * 128
                        I("pool", "dma_start", out=out_d[r0:r0 + 128, :], in_=ob[:], r=[kob], dma=True)
        P.emit()
    return nc, P


def _chunkT(v):
    return np.ascontiguousarray(np.asarray(v, np.float32).reshape(-1, 128).T)


def make_in_maps(inp):
    x = np.asarray(inp["x"], np.float32)
    sw = np.zeros((128, 128), np.float32)
    sw[:, O_G1:O_G1 + 16] = _chunkT(inp["norm1_g"][0])
    sw[:, O_G2:O_G2 + 16] = _chunkT(inp["norm2_g"][0])
    sw[:, O_PB:O_PB + 8] = _chunkT(inp["pool_b"][0].reshape(-1))
    sw[:, O_PSC:O_PSC + 8] = _chunkT(inp["pool_scale"][0])
    for k in range(4):
        sw[:, O_CW + k * 8:O_CW + (k + 1) * 8] = _chunkT(inp["conv_w"][0][k])
    sw[:, O_CB:O_CB + 8] = _chunkT(inp["conv_b"][0])
    sw[:, O_GAB:O_GAB + 8] = _chunkT(inp["gate_a_b"][0].reshape(-1))
    sw[:, O_GXB:O_GXB + 8] = _chunkT(inp["gate_x_b"][0].reshape(-1))
    sw[:, O_LAM:O_LAM + 8] = _chunkT(inp["lru_lambda"][0])
    gf = np.ascontiguousarray(np.broadcast_to(np.asarray(inp["norm_f_g"], np.float32)[None, :], (128, D)))
    shared = {
        "gf": gf,
        "w_in": np.ascontiguousarray(inp["w_in"][0], dtype=np.float32),
        "pool_w": np.ascontiguousarray(inp["pool_w"][0], dtype=np.float32),
        "gate_a_w": np.ascontiguousarray(inp["gate_a_w"][0], dtype=np.float32),
        "gate_x_w": np.ascontiguousarray(inp["gate_x_w"][0], dtype=np.float32),
        "w_out": np.ascontiguousarray(inp["w_out"][0], dtype=np.float32),
        "peer_wq": np.ascontiguousarray(inp["peer_wq"][0], dtype=np.float32),
        "keys1": np.ascontiguousarray(inp["peer_keys1"][0], dtype=np.float32),
        "keys2": np.ascontiguousarray(inp["peer_keys2"][0], dtype=np.float32),
        "peer_u": np.ascontiguousarray(inp["peer_u"][0], dtype=np.float32),
        "peer_v": np.ascontiguousarray(inp["peer_v"][0], dtype=np.float32),
    }
    zeros = np.zeros((NTOK, D), np.float32)
    maps = []
    for core in range(8):
        b, half = core // 2, core % 2
        m = dict(shared)
        m["xo"] = np.ascontiguousarray(x[b, half * NTOK:(half + 1) * NTOK])
        m["xp"] = np.ascontiguousarray(x[b, 0:NTOK]) if half == 1 else zeros
        s = sw.copy()
        s[:, O_FLAG] = float(half)
        m["smallw"] = s
        pos = half * NTOK + np.arange(16, dtype=np.float32) + 1.0
        inv = np.stack([1.0 / np.minimum(pos, float(w)) for w in (2, 4, 8, 16)], 0).astype(np.float32)
        m["invdiv"] = np.ascontiguousarray(np.broadcast_to(inv[None], (128, 4, 16)))
        maps.append(m)
    return maps


def kernel(**inputs):
    nc, _ = build()
    maps = make_in_maps(inputs)
    res = run_bass_kernel_spmd(nc, maps, core_ids=list(range(8)))
    out = np.zeros((4, 8192, D), np.float32)
    for core in range(8):
        b, half = core // 2, core % 2
        out[b, half * NTOK:(half + 1) * NTOK] = np.asarray(res.results[core]["out"], np.float32)
    return out
```
